# Optimizing a Trainium2 kernel written in Bass

```python
import math
import jax, jax.numpy as jnp
from jax import lax
import numpy as np

D_MODEL = 1024
BATCH = 8
SEQ = 2048
DEPTH = 1

SSM_GROUP = 16
SSM_GROUPS = 32
SSM_WIDTH = SSM_GROUP * SSM_GROUPS
SSM_STATE = 64
SSM_DT_MIN = 1e-3
SSM_DT_MAX = 1e-1
ATT_HEADS = 8
HEAD_DIM = 64
ATT_WIDTH = ATT_HEADS * HEAD_DIM
MOBA_BLOCK = 256
MOBA_TOPK = 3
ATT_Q_CHUNK = 32
REL_BUCKETS = 32
REL_MAX_DIST = 128
PEER_HEADS = 8
PEER_NKEYS = 128
PEER_EXPERTS = PEER_NKEYS * PEER_NKEYS
PEER_QDIM = 256
PEER_TOPK = 16
PEER_TOK_CHUNK = 128
RMS_EPS = 1e-6
NEG = -1e30
IN_WIDTHS = (SSM_WIDTH, ATT_WIDTH, ATT_WIDTH, ATT_WIDTH, D_MODEL, D_MODEL)
IN_COLS = sum(IN_WIDTHS)
IN_SPLITS = tuple(int(s) for s in np.cumsum(IN_WIDTHS)[:-1])

kernel_name = "hybrid_s5_moba_peer_block"


def rms_norm(x, g):
    xf = x.astype(jnp.float32)
    y = xf * lax.rsqrt(jnp.mean(xf * xf, axis=-1, keepdims=True) + RMS_EPS)
    return (y * g.astype(jnp.float32)).astype(x.dtype)


def _ssm_combine(c1, c2):
    a1, b1 = c1
    a2, b2 = c2
    return a1 * a2, a2 * b1 + b2


def s5_branch(u, a_re, a_im, log_dt, b_re, b_im, c_re, c_im, d_skip, w_glu, b_glu):
    f32 = jnp.float32
    bsz, seq, _ = u.shape
    uf = u.astype(f32).reshape(bsz, seq, SSM_GROUPS, SSM_GROUP)
    lam = lax.complex(a_re.astype(f32), a_im.astype(f32))
    dt = jnp.exp(log_dt.astype(f32))[:, None]
    lam_bar = jnp.exp(lam * dt)
    bmat = lax.complex(b_re.astype(f32), b_im.astype(f32))
    b_bar = ((lam_bar - 1.0) / lam)[..., None] * bmat
    cmat = lax.complex(c_re.astype(f32), c_im.astype(f32))
    bu = jnp.einsum('gph,blgh->blgp', b_bar, uf)
    a = jnp.broadcast_to(lam_bar, bu.shape)
    _, states = lax.associative_scan(_ssm_combine, (a, bu), axis=1)
    y = jnp.einsum('ghp,blgp->blgh', cmat, states).real + d_skip.astype(f32) * uf
    y = jax.nn.gelu(y.reshape(bsz, seq, SSM_WIDTH)).astype(u.dtype)
    return y * jax.nn.sigmoid(y @ w_glu + b_glu)


def t5_bucket(rel):
    n = jnp.maximum(rel, 0)
    max_exact = REL_BUCKETS // 2
    nf = jnp.maximum(n, 1).astype(jnp.float32)
    large = max_exact + (jnp.log(nf / max_exact) / math.log(REL_MAX_DIST / max_exact)
                         * (REL_BUCKETS - max_exact)).astype(jnp.int32)
    large = jnp.clip(large, 0, REL_BUCKETS - 1)
    return jnp.where(n < max_exact, n, large)


def moba_branch(q, k, v, rel_bias):
    f32 = jnp.float32
    bsz, seq, _ = q.shape
    nb = -(-seq // MOBA_BLOCK)
    lp = nb * MOBA_BLOCK

    def heads(t):
        t = t.reshape(bsz, seq, ATT_HEADS, HEAD_DIM).transpose(0, 2, 1, 3)
        return jnp.pad(t, ((0, 0), (0, 0), (0, lp - seq), (0, 0)))

    qh, kh, vh = heads(q), heads(k), heads(v)
    kb = kh.reshape(bsz, ATT_HEADS, nb, MOBA_BLOCK, HEAD_DIM)
    vb = vh.reshape(bsz, ATT_HEADS, nb, MOBA_BLOCK, HEAD_DIM)

    kmean = jnp.mean(kb.astype(f32), axis=3)
    gate = jnp.einsum('bhqd,bhnd->bhqn', qh.astype(f32), kmean)
    qblk = jnp.arange(lp) // MOBA_BLOCK
    past = jnp.arange(nb)[None, :] < qblk[:, None]
    gate = jnp.where(past, gate, -jnp.inf)
    topk = min(MOBA_TOPK, nb)
    _, sel = lax.top_k(gate, topk)

    n_chunks = lp // ATT_Q_CHUNK
    qc = qh.reshape(bsz, ATT_HEADS, n_chunks, ATT_Q_CHUNK, HEAD_DIM).transpose(2, 0, 1, 3, 4)
    selc = sel.reshape(bsz, ATT_HEADS, n_chunks, ATT_Q_CHUNK, topk).transpose(2, 0, 1, 3, 4)
    starts = jnp.arange(n_chunks, dtype=jnp.int32) * ATT_Q_CHUNK

    bi = jnp.arange(bsz)[:, None, None, None]
    hi = jnp.arange(ATT_HEADS)[None, :, None, None]
    hi5 = hi[..., None]
    table = rel_bias.astype(f32).T
    offs = jnp.arange(MOBA_BLOCK)
    scale = HEAD_DIM ** -0.5

    def step(args):
        q_c, sel_c, start = args
        qp = start + jnp.arange(ATT_Q_CHUNK)
        own = start // MOBA_BLOCK
        q_f = q_c.astype(f32)
        k_sel = kb[bi, hi, sel_c].astype(f32)
        v_sel = vb[bi, hi, sel_c].astype(f32)
        kpos_sel = sel_c[..., None] * MOBA_BLOCK + offs
        bias_sel = table[hi5, t5_bucket(qp[:, None, None] - kpos_sel)]
        s_sel = jnp.einsum('bhqd,bhqskd->bhqsk', q_f, k_sel) * scale + bias_sel
        s_sel = jnp.where((sel_c < own)[..., None], s_sel, NEG)
        k_own = lax.dynamic_index_in_dim(kb, own, axis=2, keepdims=False).astype(f32)
        v_own = lax.dynamic_index_in_dim(vb, own, axis=2, keepdims=False).astype(f32)
        rel_own = qp[:, None] - (own * MOBA_BLOCK + offs)[None, :]
        s_own = jnp.einsum('bhqd,bhkd->bhqk', q_f, k_own) * scale + table[:, t5_bucket(rel_own)]
        s_own = jnp.where(rel_own >= 0, s_own, NEG)
        s = jnp.concatenate([s_sel.reshape(bsz, ATT_HEADS, ATT_Q_CHUNK, topk * MOBA_BLOCK), s_own], axis=-1)
        p = jax.nn.softmax(s, axis=-1)
        p_sel = p[..., :topk * MOBA_BLOCK].reshape(bsz, ATT_HEADS, ATT_Q_CHUNK, topk, MOBA_BLOCK)
        p_own = p[..., topk * MOBA_BLOCK:]
        o = (jnp.einsum('bhqsk,bhqskd->bhqd', p_sel, v_sel)
             + jnp.einsum('bhqk,bhkd->bhqd', p_own, v_own))
        return o.astype(q_c.dtype)

    out = lax.map(step, (qc, selc, starts))
    out = out.transpose(1, 2, 0, 3, 4).reshape(bsz, ATT_HEADS, lp, HEAD_DIM)[:, :, :seq]
    return out.transpose(0, 2, 1, 3).reshape(bsz, seq, ATT_WIDTH)


def peer_ffn(h, w_q, sub_k1, sub_k2, u_emb, v_emb):
    f32 = jnp.float32
    bsz, seq, d = h.shape
    t = h.reshape(-1, d)
    n_tok = t.shape[0]
    half = PEER_QDIM // 2
    q = (t @ w_q).astype(f32).reshape(n_tok, PEER_HEADS, 2, half)
    s1 = jnp.einsum('thd,kd->thk', q[:, :, 0], sub_k1.astype(f32))
    s2 = jnp.einsum('thd,kd->thk', q[:, :, 1], sub_k2.astype(f32))
    v1, i1 = lax.top_k(s1, PEER_TOPK)
    v2, i2 = lax.top_k(s2, PEER_TOPK)
    n_cand = PEER_TOPK * PEER_TOPK
    cand = (v1[..., :, None] + v2[..., None, :]).reshape(n_tok, PEER_HEADS, n_cand)
    cand_idx = (i1[..., :, None] * PEER_NKEYS + i2[..., None, :]).reshape(n_tok, PEER_HEADS, n_cand)
    best, pos = lax.top_k(cand, PEER_TOPK)
    expert = jnp.take_along_axis(cand_idx, pos, axis=-1)
    gate = jax.nn.softmax(best, axis=-1)

    n_chunks = n_tok // PEER_TOK_CHUNK

    def step(args):
        t_c, e_c, g_c = args
        u_sel = jnp.take(u_emb, e_c, axis=0).astype(f32)
        act = jax.nn.gelu(jnp.einsum('cd,chkd->chk', t_c.astype(f32), u_sel))
        v_sel = jnp.take(v_emb, e_c, axis=0).astype(f32)
        return jnp.einsum('chk,chkd->cd', g_c * act, v_sel).astype(t_c.dtype)

    y = lax.map(step, (t.reshape(n_chunks, PEER_TOK_CHUNK, d),
                       expert.reshape(n_chunks, PEER_TOK_CHUNK, PEER_HEADS, PEER_TOPK),
                       gate.reshape(n_chunks, PEER_TOK_CHUNK, PEER_HEADS, PEER_TOPK)))
    return y.reshape(bsz, seq, d)


def setup_inputs(seed: int = 0) -> dict:
    key = jax.random.key(seed)
    ks = jax.random.split(key, 26)
    f32 = jnp.float32

    def nrm(k, shape, s):
        return jax.random.normal(k, shape, f32) * s

    G, P, H = SSM_GROUPS, SSM_STATE, SSM_GROUP
    n_idx = jnp.arange(SSM_STATE, dtype=f32)
    return {
        "x": nrm(ks[0], (BATCH, SEQ, D_MODEL), 1.0),
        "norm1_g": 1.0 + nrm(ks[1], (DEPTH, D_MODEL), 0.05),
        "w_in": nrm(ks[2], (DEPTH, D_MODEL, IN_COLS), D_MODEL ** -0.5),
        "ssm_a_re": -0.5 + nrm(ks[3], (DEPTH, G, P), 0.01),
        "ssm_a_im": math.pi * n_idx + nrm(ks[4], (DEPTH, G, P), 0.01),
        "ssm_log_dt": jax.random.uniform(ks[5], (DEPTH, G), f32, math.log(SSM_DT_MIN), math.log(SSM_DT_MAX)),
        "ssm_b_re": nrm(ks[6], (DEPTH, G, P, H), (2 * H) ** -0.5),
        "ssm_b_im": nrm(ks[7], (DEPTH, G, P, H), (2 * H) ** -0.5),
        "ssm_c_re": nrm(ks[8], (DEPTH, G, H, P), (2 * P) ** -0.5),
        "ssm_c_im": nrm(ks[9], (DEPTH, G, H, P), (2 * P) ** -0.5),
        "ssm_d": nrm(ks[10], (DEPTH, G, H), 1.0),
        "ssm_w_glu": nrm(ks[11], (DEPTH, SSM_WIDTH, SSM_WIDTH), SSM_WIDTH ** -0.5),
        "ssm_b_glu": nrm(ks[12], (DEPTH, SSM_WIDTH), 0.01),
        "w_up_ssm": nrm(ks[13], (DEPTH, SSM_WIDTH, D_MODEL), SSM_WIDTH ** -0.5),
        "w_up_att": nrm(ks[14], (DEPTH, ATT_WIDTH, D_MODEL), ATT_WIDTH ** -0.5),
        "rel_bias": nrm(ks[15], (REL_BUCKETS, ATT_HEADS), 0.5),
        "w_out": nrm(ks[16], (DEPTH, D_MODEL, D_MODEL), D_MODEL ** -0.5),
        "norm2_g": 1.0 + nrm(ks[17], (DEPTH, D_MODEL), 0.05),
        "peer_w_q": nrm(ks[18], (DEPTH, D_MODEL, PEER_HEADS * PEER_QDIM), D_MODEL ** -0.5),
        "peer_sub_k1": nrm(ks[19], (DEPTH, PEER_NKEYS, PEER_QDIM // 2), (PEER_QDIM // 2) ** -0.5),
        "peer_sub_k2": nrm(ks[20], (DEPTH, PEER_NKEYS, PEER_QDIM // 2), (PEER_QDIM // 2) ** -0.5),
        "peer_u": nrm(ks[21], (DEPTH, PEER_EXPERTS, D_MODEL), D_MODEL ** -0.5),
        "peer_v": nrm(ks[22], (DEPTH, PEER_EXPERTS, D_MODEL), PEER_HEADS ** -0.5),
        "final_g": 1.0 + nrm(ks[23], (D_MODEL,), 0.05),
    }


def reference(x, norm1_g, w_in, ssm_a_re, ssm_a_im, ssm_log_dt, ssm_b_re, ssm_b_im, ssm_c_re, ssm_c_im,
              ssm_d, ssm_w_glu, ssm_b_glu, w_up_ssm, w_up_att, rel_bias, w_out, norm2_g, peer_w_q,
              peer_sub_k1, peer_sub_k2, peer_u, peer_v, final_g):
    for l in range(DEPTH):
        h = rms_norm(x, norm1_g[l])
        z = h @ w_in[l]
        u_ssm, q, k, v, g_a, g_b = jnp.split(z, IN_SPLITS, axis=-1)
        y_a = s5_branch(u_ssm, ssm_a_re[l], ssm_a_im[l], ssm_log_dt[l], ssm_b_re[l], ssm_b_im[l],
                        ssm_c_re[l], ssm_c_im[l], ssm_d[l], ssm_w_glu[l], ssm_b_glu[l])
        y_b = moba_branch(q, k, v, rel_bias)
        merged = (jax.nn.sigmoid(g_a) * (y_a @ w_up_ssm[l])
                  + jax.nn.sigmoid(g_b) * (y_b @ w_up_att[l]))
        x = x + merged @ w_out[l]
        x = x + peer_ffn(rms_norm(x, norm2_g[l]), peer_w_q[l], peer_sub_k1[l], peer_sub_k2[l],
                         peer_u[l], peer_v[l])
    return rms_norm(x, final_g)
```

```python
from contextlib import ExitStack
import numpy as np
import ml_dtypes
import concourse.bass as bass
import concourse.mybir as mybir
from concourse.bass_utils import run_bass_kernel_spmd

F32 = mybir.dt.float32
BF16 = mybir.dt.bfloat16
U32 = mybir.dt.uint32
ALU = mybir.AluOpType
AF = mybir.ActivationFunctionType
AX = mybir.AxisListType

D = 1024
L = 2048
NT = 16
NEG = -30000.0
PI = float(np.pi)


class Prog:
    ENGS = ("tensor", "vector", "scalar", "gpsimd", "sync")
    EPOCH = 4000
    DEPOCH = 250

    def __init__(self, nc, es):
        self.nc = nc
        self.es = es
        self.streams = {e: [] for e in self.ENGS}
        self.cnt = {e: 0 for e in self.ENGS}
        self.lastw = {}
        self.readers = {}
        self.waited = {}
        self.sems = {}
        self.dcnt = {}
        self.pending = {e: [] for e in self.ENGS}
        self.psum = set()

    def sem(self, key):
        if key not in self.sems:
            self.sems[key] = self.es.enter_context(self.nc.semaphore(f"sm{len(self.sems)}"))
        return self.sems[key]

    def _wait_for(self, sig):
        if sig[0] == "E":
            _, e2, n = sig
            ep = (n - 1) // self.EPOCH
            return ("E", e2, ep), n - ep * self.EPOCH
        _, key, n = sig
        ep = (n - 1) // self.DEPOCH
        return ("D", key, ep), 16 * (n - ep * self.DEPOCH)

    def op(self, eng, fn, r=(), w=(), dma=None):
        w = list(w) + [b for b in r if b.split(":")[0] in self.psum and b not in w]
        deps = set()
        for b in r:
            if b in self.lastw:
                deps.add(self.lastw[b])
        for b in w:
            if b in self.lastw:
                s = self.lastw[b]
                if not (s[0] == "E" and s[1] == eng and eng == "tensor"):
                    deps.add(s)
            for s in self.readers.get(b, ()):
                if not (s[0] == "E" and s[1] == eng and eng == "tensor"):
                    deps.add(s)
        waits = list(self.pending[eng])
        self.pending[eng] = []
        for s in deps:
            waits.append(self._wait_for(s))
        fw = []
        for k, v in waits:
            if self.waited.get((eng, k), 0) >= v:
                continue
            self.waited[(eng, k)] = v
            fw.append((k, v))
        if dma is None:
            self.cnt[eng] += 1
            n = self.cnt[eng]
            sig = ("E", eng, n)
            ep = (n - 1) // self.EPOCH
            inc = (("E", eng, ep), 1)
        else:
            self.dcnt[dma] = self.dcnt.get(dma, 0) + 1
            n = self.dcnt[dma]
            sig = ("D", dma, n)
            ep = (n - 1) // self.DEPOCH
            inc = (("D", dma, ep), 16)
        for k, _ in fw:
            self.sem(k)
        self.sem(inc[0])
        self.streams[eng].append((fw, fn, inc))
        for b in r:
            self.readers.setdefault(b, []).append(sig)
        for b in w:
            self.lastw[b] = sig
            self.readers[b] = []
        return sig

    def barrier(self):
        sigs = []
        for e in self.ENGS:
            if self.cnt[e] > 0:
                sigs.append(("E", e, self.cnt[e]))
        for k, n in self.dcnt.items():
            sigs.append(("D", k, n))
        for e in self.ENGS:
            for s in sigs:
                if s[0] == "E" and s[1] == e:
                    continue
                self.pending[e].append(self._wait_for(s))

    def emit(self, final=False):
        nc = self.nc
        if not final:
            return
        if final:
            self.barrier()
            for e in self.ENGS:
                self.op(e, lambda en: en.nop())
        streams = self.streams
        sems = self.sems

        def run(name, en):
            for waits, fn, inc in streams[name]:
                for k, v in waits:
                    en.wait_ge(sems[k], v)
                ins = fn(en)
                ins.then_inc(sems[inc[0]], inc[1])

        with nc.Block() as block:
            @block.tensor
            def _(en):
                run("tensor", en)

            @block.vector
            def _(en):
                run("vector", en)

            @block.scalar
            def _(en):
                run("scalar", en)

            @block.gpsimd
            def _(en):
                run("gpsimd", en)

            @block.sync
            def _(en):
                run("sync", en)
        self.streams = {e: [] for e in self.ENGS}


def build(debug=None, stop=99):
    nc = bass.Bass("TRN2", target_bir_lowering=False)
    es = ExitStack()
    P = Prog(nc, es)

    def din(name, shape, dt=F32):
        return nc.dram_tensor(name, list(shape), dt, kind="ExternalInput").ap()

    x = din("x", [L, D])
    norm1_g = din("norm1_g", [1, D])
    final_g = din("final_g", [1, D])
    out = nc.dram_tensor("out", [L, D], F32, kind="ExternalOutput").ap()
    dbg = None
    if debug is not None:
        dbg = nc.dram_tensor("dbg", list(debug), F32, kind="ExternalOutput").ap()

    def sb(name, shape, dt=F32):
        return es.enter_context(nc.sbuf_tensor(name, list(shape), dt))

    def ps(name, shape, dt=F32):
        P.psum.add(name)
        return es.enter_context(nc.psum_tensor(name, list(shape), dt))

    ident_b = sb("ident_b", [128, 128], BF16)
    ident_f = sb("ident_f", [128, 128], F32)
    ss = sb("ss", [128, NT])
    rstd = sb("rstd", [128, NT])
    mergedT = sb("mergedT", [128, 8, L], BF16)
    MID = ExitStack()
    hT = MID.enter_context(nc.sbuf_tensor("hT", [128, 8, L], BF16))
    yagT = MID.enter_context(nc.sbuf_tensor("yagT", [128, 4, L], BF16))
    P1 = ExitStack()
    g1 = P1.enter_context(nc.sbuf_tensor("g1", [128, D], F32))
    xt = [P1.enter_context(nc.sbuf_tensor(f"xt{i}", [128, D], F32)) for i in range(2)]
    junk = P1.enter_context(nc.sbuf_tensor("junk", [128, D], F32))
    hb = [P1.enter_context(nc.sbuf_tensor(f"hb{i}", [128, D], BF16)) for i in range(2)]
    P.psum.update(["pT0", "pT1"])
    pT = [P1.enter_context(nc.psum_tensor(f"pT{i}", [128, 8, 128], BF16)) for i in range(2)]

    P.op("gpsimd", lambda e: e.memset(ident_f[:], 0.0), w=["ident_f"])
    P.op("gpsimd", lambda e: e.affine_select(out=ident_f[:], in_=ident_f[:], pattern=[[-1, 128]],
                                             compare_op=ALU.not_equal, fill=1.0, base=0,
                                             channel_multiplier=1), r=["ident_f"], w=["ident_f"])
    P.op("vector", lambda e: e.tensor_copy(out=ident_b[:], in_=ident_f[:]), r=["ident_f"], w=["ident_b"])
    P.op("sync", lambda e: e.dma_start(out=g1[:], in_=norm1_g.partition_broadcast(128)), w=["g1"], dma="g1")
    P.op("vector", lambda e: e.memset(ss[:], 0.0), w=[f"ss{t}" for t in range(NT)])
    peer_u = din("peer_u", [16384, D])
    peer_v = din("peer_v", [16384, D])
    uvb = nc.dram_tensor("uv_scr", [16384, 2 * D], BF16, kind="Internal").ap()
    for q in range(16):
        P.op("gpsimd", lambda e, q=q: e.dma_start(out=uvb[q * 1024:(q + 1) * 1024, 0:D], in_=peer_u[q * 1024:(q + 1) * 1024, :]), w=["ubv"], dma="ubv")
        P.op("gpsimd", lambda e, q=q: e.dma_start(out=uvb[q * 1024:(q + 1) * 1024, D:2 * D], in_=peer_v[q * 1024:(q + 1) * 1024, :]), w=["ubv"], dma="ubv")

    for tt in range(NT):
        s = tt % 2
        P.op("sync", lambda e, tt=tt, s=s: e.dma_start(out=xt[s][:], in_=x[tt * 128:(tt + 1) * 128, :]),
             w=[f"xt{s}"], dma=f"xt{s}")
        P.op("scalar", lambda e, tt=tt, s=s: e.activation(out=junk[:], in_=xt[s][:], func=AF.Square,
                                                          accum_out=ss[:, tt:tt + 1]),
             r=[f"xt{s}"], w=["junk", f"ss{tt}"])
        P.op("vector", lambda e, tt=tt: e.tensor_scalar(out=rstd[:, tt:tt + 1], in0=ss[:, tt:tt + 1],
                                                        scalar1=1.0 / D, scalar2=1e-6, op0=ALU.mult, op1=ALU.add),
             r=[f"ss{tt}"], w=[f"rstd{tt}"])
        P.op("scalar", lambda e, tt=tt: e.activation(out=rstd[:, tt:tt + 1], in_=rstd[:, tt:tt + 1], func=AF.Sqrt),
             r=[f"rstd{tt}"], w=[f"rstd{tt}"])
        P.op("vector", lambda e, tt=tt: e.reciprocal(out=rstd[:, tt:tt + 1], in_=rstd[:, tt:tt + 1]),
             r=[f"rstd{tt}"], w=[f"rstd{tt}"])
        P.op("vector", lambda e, tt=tt, s=s: e.scalar_tensor_tensor(out=hb[s][:], in0=xt[s][:], scalar=rstd[:, tt:tt + 1],
                                                                    in1=g1[:], op0=ALU.mult, op1=ALU.mult),
             r=[f"xt{s}", f"rstd{tt}", "g1"], w=[f"hb{s}"])
        for dk in range(8):
            P.op("tensor", lambda e, s=s, dk=dk: e.transpose(out=pT[s][:, dk, :], in_=hb[s][:, dk * 128:(dk + 1) * 128],
                                                             identity=ident_b[:]),
                 r=[f"hb{s}", "ident_b"], w=[f"pT{s}"])
        P.op("scalar", lambda e, tt=tt, s=s: e.copy(out=hT[:, :, tt * 128:(tt + 1) * 128], in_=pT[s][:]),
             r=[f"pT{s}"], w=[f"hT{tt}"])


    if stop == 1:
        for dk in range(8):
            P.op("vector", lambda e, dk=dk: e.tensor_copy(out=junk[:, 0:1024], in_=hT[:, dk, 0:1024]),
                 r=[f"hT{t}" for t in range(NT)] + ["junk"], w=["junk"])
            P.op("sync", lambda e, dk=dk: e.dma_start(out=dbg[:, dk * 1024:(dk + 1) * 1024], in_=junk[:, 0:1024]),
                 r=["junk"], w=[], dma="dbgout")
        P.emit(final=True)
        return nc
    P.barrier()
    P.emit()
    P1.close()

    w_in = din("w_in", [D, 4096])
    s5_lp = din("s5_lp", [128, 48])
    s5_row = din("s5_row", [128, 3, 2048])
    s5_braw = din("s5_braw", [128, 2, 2048])
    s5_cpad = din("s5_cpad", [128, 2, 2048])
    s5_d = din("s5_d", [128, 4])
    iota_ab = din("iota_ab", [128, 96])

    YA = ExitStack()
    yaT = YA.enter_context(nc.sbuf_tensor("yaT", [128, 4, L], BF16))
    S5 = ExitStack()

    def sbp(name, shape, dt=F32):
        return S5.enter_context(nc.sbuf_tensor(name, list(shape), dt))

    def psp(name, shape, dt=F32):
        P.psum.add(name)
        return S5.enter_context(nc.psum_tensor(name, list(shape), dt))

    Bp = [sbp(f"Bp{i}", [128, 16, 128], BF16) for i in range(2)]
    Cp = [sbp(f"Cp{i}", [128, 16, 128], BF16) for i in range(2)]
    lp = sbp("lp", [128, 48])
    rho = sbp("rho", [128, 16])
    th = sbp("th", [128, 16])
    th64 = sbp("th64", [128, 16])
    dcol = sbp("dcol", [128, 4])
    iab = sbp("iab", [128, 96])
    ang1 = sbp("ang1", [128, 16, 32])
    thb = sbp("thb", [128, 16, 64])
    MAGIC = 12582912.0
    I2PI = float(1.0 / (2 * np.pi))
    TWOPI = float(2 * np.pi)

    def reduce_angle(eng, dst, src, tmp, rk, wk, tk, shift=0.0):
        P.op(eng, lambda e: e.tensor_scalar(out=tmp, in0=src, scalar1=I2PI, scalar2=MAGIC + shift * I2PI,
                                            op0=ALU.mult, op1=ALU.add), r=rk, w=tk)
        P.op(eng, lambda e: e.tensor_scalar(out=tmp, in0=tmp, scalar1=-MAGIC, scalar2=-TWOPI,
                                            op0=ALU.add, op1=ALU.mult), r=tk, w=tk)
        P.op(eng, lambda e: e.tensor_tensor(out=dst, in0=src, in1=tmp, op=ALU.add), r=list(rk) + list(tk), w=wk)
        if shift != 0.0:
            P.op(eng, lambda e: e.tensor_scalar(out=dst, in0=dst, scalar1=shift, scalar2=None, op0=ALU.add), r=wk, w=wk)

    P.op("sync", lambda e: e.dma_start(out=lp[:], in_=s5_lp), w=["lp"], dma="lp")
    P.op("sync", lambda e: e.dma_start(out=dcol[:], in_=s5_d), w=["dcol"], dma="dcol")
    P.op("sync", lambda e: e.dma_start(out=iab[:], in_=iota_ab), w=["iab"], dma="iab")

    with ExitStack() as PR:
        def sbr(name, shape, dt=F32):
            return PR.enter_context(nc.sbuf_tensor(name, list(shape), dt))
        rowp = sbr("rowp", [128, 3, 2048])
        braw = sbr("braw", [128, 2, 2048])
        tA = sbr("tA", [128, 2048]); tB = sbr("tB", [128, 2048]); tC = sbr("tC", [128, 2048])
        tD = sbr("tD", [128, 2048]); tE = sbr("tE", [128, 2048]); tF = sbr("tF", [128, 2048])
        dtl = sbr("dtl", [128, 16]); tl = sbr("tl", [128, 16]); tl2 = sbr("tl2", [128, 16])
        P.op("sync", lambda e: e.dma_start(out=rowp[:], in_=s5_row), w=["rowp"], dma="rowp")
        P.op("sync", lambda e: e.dma_start(out=braw[:], in_=s5_braw), w=["braw"], dma="braw")
        P.op("sync", lambda e: e.dma_start(out=tE[:], in_=s5_cpad[:, 0, :]), w=["tE"], dma="crawE")
        P.op("sync", lambda e: e.dma_start(out=tF[:], in_=s5_cpad[:, 1, :]), w=["tF"], dma="crawF")
        P.op("vector", lambda e: e.tensor_copy(out=Cp[0][:].rearrange("p a b -> p (a b)"), in_=tE[:]), r=["tE"], w=["Cp0"])
        P.op("vector", lambda e: e.tensor_copy(out=Cp[1][:].rearrange("p a b -> p (a b)"), in_=tF[:]), r=["tF"], w=["Cp1"])
        P.op("scalar", lambda e: e.activation(out=dtl[:], in_=lp[:, 32:48], func=AF.Exp), r=["lp"], w=["dtl"])
        P.op("vector", lambda e: e.tensor_tensor(out=tl[:], in0=dtl[:], in1=lp[:, 0:16], op=ALU.mult), r=["dtl", "lp"], w=["tl"])
        P.op("scalar", lambda e: e.activation(out=rho[:], in_=tl[:], func=AF.Exp), r=["tl"], w=["rho"])
        P.op("vector", lambda e: e.tensor_tensor(out=tl[:], in0=dtl[:], in1=lp[:, 16:32], op=ALU.mult), r=["dtl", "lp", "rho"], w=["tl"])
        reduce_angle("vector", th[:], tl[:], tl2[:], ["tl"], ["th"], ["tl2"])
        P.op("vector", lambda e: e.tensor_scalar(out=tl[:], in0=th[:], scalar1=64.0, scalar2=None, op0=ALU.mult), r=["th"], w=["tl"])
        reduce_angle("vector", th64[:], tl[:], tl2[:], ["tl"], ["th64"], ["tl2"])
        for pr in range(16):
            P.op("vector", lambda e, pr=pr: e.tensor_scalar(out=ang1[:, pr, :], in0=iab[:, 0:32], scalar1=th64[:, pr:pr + 1],
                                                            scalar2=None, op0=ALU.mult), r=["iab", "th64"], w=["ang1"])
            P.op("vector", lambda e, pr=pr: e.tensor_scalar(out=thb[:, pr, :], in0=iab[:, 32:96], scalar1=th[:, pr:pr + 1],
                                                            scalar2=None, op0=ALU.mult), r=["iab", "th"], w=["thb"])
        a1f = ang1[:].rearrange("p a b -> p (a b)")
        reduce_angle("vector", a1f, a1f, tA[:, 0:512], ["ang1"], ["ang1"], ["tA"])
        are_r = rowp[:, 0, :]; aim_r = rowp[:, 1, :]
        P.op("scalar", lambda e: e.activation(out=tA[:], in_=rowp[:, 2, :], func=AF.Exp), r=["rowp", "tA"], w=["tA"])
        P.op("vector", lambda e: e.tensor_tensor(out=tB[:], in0=tA[:], in1=are_r, op=ALU.mult), r=["tA", "rowp"], w=["tB"])
        P.op("scalar", lambda e: e.activation(out=tB[:], in_=tB[:], func=AF.Exp), r=["tB"], w=["tB"])
        P.op("vector", lambda e: e.tensor_tensor(out=tC[:], in0=tA[:], in1=aim_r, op=ALU.mult), r=["tA", "rowp"], w=["tC"])
        reduce_angle("vector", tD[:], tC[:], tE[:], ["tC"], ["tD"], ["tE"])
        reduce_angle("vector", tF[:], tC[:], tE[:], ["tC"], ["tF"], ["tE"], shift=PI / 2)
        P.op("scalar", lambda e: e.activation(out=tD[:], in_=tD[:], func=AF.Sin), r=["tD"], w=["tD"])
        P.op("scalar", lambda e: e.activation(out=tF[:], in_=tF[:], func=AF.Sin), r=["tF"], w=["tF"])
        P.op("vector", lambda e: e.tensor_tensor(out=tF[:], in0=tF[:], in1=tB[:], op=ALU.mult), r=["tF", "tB"], w=["tF"])
        P.op("vector", lambda e: e.tensor_scalar(out=tF[:], in0=tF[:], scalar1=-1.0, scalar2=None, op0=ALU.add), r=["tF"], w=["tF"])
        P.op("vector", lambda e: e.tensor_tensor(out=tD[:], in0=tD[:], in1=tB[:], op=ALU.mult), r=["tD", "tB"], w=["tD"])
        P.op("vector", lambda e: e.tensor_tensor(out=tA[:], in0=are_r, in1=are_r, op=ALU.mult), r=["rowp", "tA"], w=["tA"])
        P.op("vector", lambda e: e.tensor_tensor(out=tC[:], in0=aim_r, in1=aim_r, op=ALU.mult), r=["rowp", "tC"], w=["tC"])
        P.op("vector", lambda e: e.tensor_tensor(out=tA[:], in0=tA[:], in1=tC[:], op=ALU.add), r=["tA", "tC"], w=["tA"])
        P.op("vector", lambda e: e.reciprocal(out=tA[:], in_=tA[:]), r=["tA"], w=["tA"])
        P.op("vector", lambda e: e.tensor_tensor(out=tB[:], in0=tF[:], in1=are_r, op=ALU.mult), r=["tF", "rowp", "tB"], w=["tB"])
        P.op("vector", lambda e: e.tensor_tensor(out=tE[:], in0=tD[:], in1=aim_r, op=ALU.mult), r=["tD", "rowp", "tE"], w=["tE"])
        P.op("vector", lambda e: e.tensor_tensor(out=tB[:], in0=tB[:], in1=tE[:], op=ALU.add), r=["tB", "tE"], w=["tB"])
        P.op("vector", lambda e: e.tensor_tensor(out=tB[:], in0=tB[:], in1=tA[:], op=ALU.mult), r=["tB", "tA"], w=["tB"])
        P.op("vector", lambda e: e.tensor_tensor(out=tC[:], in0=tD[:], in1=are_r, op=ALU.mult), r=["tD", "rowp", "tC"], w=["tC"])
        P.op("vector", lambda e: e.tensor_tensor(out=tE[:], in0=tF[:], in1=aim_r, op=ALU.mult), r=["tF", "rowp", "tE"], w=["tE"])
        P.op("vector", lambda e: e.tensor_tensor(out=tC[:], in0=tC[:], in1=tE[:], op=ALU.subtract), r=["tC", "tE"], w=["tC"])
        P.op("vector", lambda e: e.tensor_tensor(out=tC[:], in0=tC[:], in1=tA[:], op=ALU.mult), r=["tC", "tA"], w=["tC"])
        brr = braw[:, 0, :]; bri = braw[:, 1, :]
        P.op("vector", lambda e: e.tensor_tensor(out=tD[:], in0=brr, in1=tB[:], op=ALU.mult), r=["braw", "tB", "tD"], w=["tD"])
        P.op("vector", lambda e: e.tensor_tensor(out=tE[:], in0=bri, in1=tC[:], op=ALU.mult), r=["braw", "tC", "tE"], w=["tE"])
        P.op("vector", lambda e: e.tensor_tensor(out=Bp[0][:].rearrange("p a b -> p (a b)"), in0=tD[:], in1=tE[:], op=ALU.subtract),
             r=["tD", "tE"], w=["Bp0"])
        P.op("vector", lambda e: e.tensor_tensor(out=tD[:], in0=brr, in1=tC[:], op=ALU.mult), r=["braw", "tC", "tD"], w=["tD"])
        P.op("vector", lambda e: e.tensor_tensor(out=tE[:], in0=bri, in1=tB[:], op=ALU.mult), r=["braw", "tB", "tE"], w=["tE"])
        P.op("vector", lambda e: e.tensor_tensor(out=Bp[1][:].rearrange("p a b -> p (a b)"), in0=tD[:], in1=tE[:], op=ALU.add),
             r=["tD", "tE"], w=["Bp1"])
        if stop == 2:
            P.op("vector", lambda e: e.tensor_copy(out=tD[:], in_=Bp[0][:].rearrange("p a b -> p (a b)")), r=["Bp0", "tD"], w=["tD"])
            P.op("vector", lambda e: e.tensor_copy(out=tE[:], in_=Bp[1][:].rearrange("p a b -> p (a b)")), r=["Bp1", "tE"], w=["tE"])
            P.op("sync", lambda e: e.dma_start(out=dbg[:, 0:2048], in_=tD[:]), r=["tD"], dma="dbgout")
            P.op("sync", lambda e: e.dma_start(out=dbg[:, 2048:4096], in_=tE[:]), r=["tE"], dma="dbgout")
            P.op("sync", lambda e: e.dma_start(out=dbg[:, 4096:4096 + 16], in_=rho[:]), r=["rho"], dma="dbgout")
            P.op("sync", lambda e: e.dma_start(out=dbg[:, 4112:4112 + 16], in_=th[:]), r=["th"], dma="dbgout")
            P.op("sync", lambda e: e.dma_start(out=dbg[:, 4128:4128 + 16], in_=th64[:]), r=["th64"], dma="dbgout")
            P.op("sync", lambda e: e.dma_start(out=dbg[:, 4144:4144 + 512], in_=ang1[:].rearrange("p a b -> p (a b)")), r=["ang1"], dma="dbgout")
            P.emit(final=True)
            return nc
        P.barrier()
        P.emit()

    uTb = sbp("uTb", [128, 4, L], BF16)
    hkeys = [f"hT{t}" for t in range(NT)]
    with ExitStack() as US:
        wst = [US.enter_context(nc.sbuf_tensor(f"wst{i}", [128, 8, 512], F32)) for i in range(2)]
        wbf = [US.enter_context(nc.sbuf_tensor(f"wbf{i}", [128, 8, 512], BF16)) for i in range(2)]
        P.psum.update(["pu0", "pu1"])
        pu = [US.enter_context(nc.psum_tensor(f"pu{i}", [128, 512], F32)) for i in range(2)]

        def load_w(slot, src_ap, ncols, kc=8):
            for q in range(kc):
                P.op("sync", lambda e, q=q: e.dma_start(out=wst[slot][:, q, 0:ncols], in_=src_ap[q * 128:(q + 1) * 128, :]),
                     w=[f"wst{slot}"], dma=f"wst{slot}")
            P.op("vector", lambda e: e.tensor_copy(out=wbf[slot][:, 0:kc, 0:ncols], in_=wst[slot][:, 0:kc, 0:ncols]),
                 r=[f"wst{slot}"], w=[f"wbf{slot}"])

        load_w(0, w_in[:, 0:512], 512)
        k = 0
        for ct in range(4):
            for c4 in range(4):
                pz = k % 2
                k += 1
                for dk in range(8):
                    P.op("tensor", lambda e, ct=ct, c4=c4, dk=dk, pz=pz: e.matmul(
                        pu[pz][:], lhsT=wbf[0][:, dk, ct * 128:(ct + 1) * 128], rhs=hT[:, dk, c4 * 512:(c4 + 1) * 512],
                        start=(dk == 0), stop=(dk == 7)), r=["wbf0"] + hkeys[c4 * 4:c4 * 4 + 4], w=[f"pu{pz}"])
                P.op("scalar", lambda e, ct=ct, c4=c4, pz=pz: e.copy(out=uTb[:, ct, c4 * 512:(c4 + 1) * 512], in_=pu[pz][:]),
                     r=[f"pu{pz}"], w=[f"uTb{ct}"])
        P.barrier()
        P.emit()

    with ExitStack() as SC:
        def sbc(name, shape, dt=F32):
            return SC.enter_context(nc.sbuf_tensor(name, list(shape), dt))

        def psc(name, shape, dt=F32):
            P.psum.add(name)
            return SC.enter_context(nc.psum_tensor(name, list(shape), dt))
        scr = mergedT[:].rearrange("p a b -> p (a b)").bitcast(F32)
        sinT = [sbc("sinT", [128, 2048])[:], scr[:, 0:2048]]
        cosT = [sbc("cosT", [128, 2048])[:], scr[:, 2048:4096]]
        angE = scr[:, 4096:6144]
        tmpF = scr[:, 6144:8192]
        halfpi = sbc("halfpi", [128, 1])
        P.op("vector", lambda e: e.memset(halfpi[:], PI / 2), w=["halfpi"])
        Sr = sbc("Sr", [128, 2048]); Si = sbc("Si", [128, 2048])
        wr = sbc("wr", [128, 2048]); wi = sbc("wi", [128, 2048])
        Xr = [sbc(f"Xr{i}", [128, 2048], BF16) for i in range(2)]
        nXi = [sbc(f"nXi{i}", [128, 2048], BF16) for i in range(2)]
        ypre = Sr; yt2 = Si
        pbr = [psc(f"pbr{i}", [128, 512]) for i in range(2)]
        pbi = [psc(f"pbi{i}", [128, 512]) for i in range(2)]
        yps = [psc(f"yps{i}", [128, 512]) for i in range(4)]
        def tables_a(pr):
            tb = pr % 2
            sT, cT = sinT[tb], cosT[tb]
            sk, ck = f"sinT{tb}", f"cosT{tb}"
            P.op("vector", lambda e, pr=pr: e.tensor_tensor(
                out=angE.rearrange("p (a b) -> p a b", a=32),
                in0=ang1[:, pr, :].unsqueeze(2).to_broadcast([128, 32, 64]),
                in1=thb[:, pr, :].unsqueeze(1).to_broadcast([128, 32, 64]), op=ALU.add),
                r=["ang1", "thb"], w=["angE"])
            reduce_angle("gpsimd", sT, angE, tmpF, ["angE"], [sk], ["tmpF"])
            P.op("scalar", lambda e, sT=sT, cT=cT: e.activation(out=cT, in_=sT, func=AF.Sin, scale=0.5), r=[sk], w=[ck])
            P.op("scalar", lambda e, sT=sT: e.activation(out=sT, in_=sT, func=AF.Sin), r=[sk], w=[sk])

        def tables_b(pr):
            tb = pr % 2
            cT = cosT[tb]
            ck = f"cosT{tb}"
            P.op("vector", lambda e, cT=cT: e.tensor_tensor(out=cT, in0=cT, in1=cT, op=ALU.mult), r=[ck], w=[ck])
            P.op("vector", lambda e, cT=cT: e.tensor_scalar(out=cT, in0=cT, scalar1=-2.0, scalar2=1.0, op0=ALU.mult, op1=ALU.add), r=[ck], w=[ck])

        tables_a(0)
        tables_b(0)
        kk = 0
        for pr in range(16):
            ct, pl = pr // 4, pr % 4
            xs = pr % 2
            tb = pr % 2
            sT, cT = sinT[tb], cosT[tb]
            sk, ck = f"sinT{tb}", f"cosT{tb}"
            for c4 in range(4):
                pz = kk % 2
                kk += 1
                sl = slice(c4 * 512, (c4 + 1) * 512)
                P.op("tensor", lambda e, pr=pr, ct=ct, sl=sl, pz=pz: e.matmul(pbr[pz][:], lhsT=Bp[0][:, pr, :], rhs=uTb[:, ct, sl],
                                                                             start=True, stop=True),
                     r=["Bp0", f"uTb{ct}"], w=[f"pbr{pz}"])
                P.op("tensor", lambda e, pr=pr, ct=ct, sl=sl, pz=pz: e.matmul(pbi[pz][:], lhsT=Bp[1][:, pr, :], rhs=uTb[:, ct, sl],
                                                                             start=True, stop=True),
                     r=["Bp1", f"uTb{ct}"], w=[f"pbi{pz}"])
                P.op("scalar", lambda e, pz=pz, sl=sl: e.copy(out=Sr[:, sl], in_=pbr[pz][:]), r=[f"pbr{pz}"], w=["Sr"])
                P.op("scalar", lambda e, pz=pz, sl=sl: e.copy(out=Si[:, sl], in_=pbi[pz][:]), r=[f"pbi{pz}"], w=["Si"])
            if pr + 1 < 16:
                tables_a(pr + 1)
            P.op("vector", lambda e, cT=cT: e.tensor_tensor(out=wr[:], in0=Sr[:], in1=cT, op=ALU.mult), r=["Sr", ck], w=["wr"])
            P.op("vector", lambda e, sT=sT: e.tensor_tensor(out=wi[:], in0=Si[:], in1=sT, op=ALU.mult), r=["Si", sk], w=["wi"])
            P.op("vector", lambda e: e.tensor_tensor(out=wr[:], in0=wr[:], in1=wi[:], op=ALU.add), r=["wr", "wi"], w=["wr"])
            P.op("vector", lambda e, cT=cT: e.tensor_tensor(out=wi[:], in0=Si[:], in1=cT, op=ALU.mult), r=["Si", ck], w=["wi"])
            P.op("vector", lambda e, sT=sT: e.tensor_tensor(out=Sr[:], in0=Sr[:], in1=sT, op=ALU.mult), r=["Sr", sk], w=["Sr"])
            P.op("vector", lambda e: e.tensor_tensor(out=wi[:], in0=wi[:], in1=Sr[:], op=ALU.subtract), r=["wi", "Sr"], w=["wi"])
            rb = rho[:, pr:pr + 1].to_broadcast([128, 2048])
            P.op("vector", lambda e, rb=rb: e.tensor_tensor_scan(out=Sr[:], data0=rb, data1=wr[:], initial=0.0,
                                                                op0=ALU.mult, op1=ALU.add), r=["rho", "wr"], w=["Sr"])
            P.op("vector", lambda e, rb=rb: e.tensor_tensor_scan(out=Si[:], data0=rb, data1=wi[:], initial=0.0,
                                                                op0=ALU.mult, op1=ALU.add), r=["rho", "wi"], w=["Si"])
            P.op("vector", lambda e, cT=cT: e.tensor_tensor(out=wr[:], in0=Sr[:], in1=cT, op=ALU.mult), r=["Sr", ck], w=["wr"])
            P.op("vector", lambda e, sT=sT: e.tensor_tensor(out=wi[:], in0=Si[:], in1=sT, op=ALU.mult), r=["Si", sk], w=["wi"])
            P.op("vector", lambda e, xs=xs: e.tensor_tensor(out=Xr[xs][:], in0=wr[:], in1=wi[:], op=ALU.subtract), r=["wr", "wi"], w=[f"Xr{xs}"])
            P.op("vector", lambda e, sT=sT: e.tensor_tensor(out=wr[:], in0=Sr[:], in1=sT, op=ALU.mult), r=["Sr", sk], w=["wr"])
            P.op("vector", lambda e, cT=cT: e.tensor_tensor(out=wi[:], in0=Si[:], in1=cT, op=ALU.mult), r=["Si", ck], w=["wi"])
            P.op("vector", lambda e, xs=xs: e.scalar_tensor_tensor(out=nXi[xs][:], in0=wr[:], scalar=-1.0, in1=wi[:],
                                                                  op0=ALU.mult, op1=ALU.subtract), r=["wr", "wi"], w=[f"nXi{xs}"])
            if pr + 1 < 16:
                tables_b(pr + 1)
            for c4 in range(4):
                sl = slice(c4 * 512, (c4 + 1) * 512)
                P.op("tensor", lambda e, pr=pr, sl=sl, xs=xs, c4=c4, pl=pl: e.matmul(yps[c4][:], lhsT=Cp[0][:, pr, :], rhs=Xr[xs][:, sl],
                                                                                    start=(pl == 0), stop=False),
                     r=["Cp0", f"Xr{xs}"], w=[f"yps{c4}"])
                P.op("tensor", lambda e, pr=pr, sl=sl, xs=xs, c4=c4, pl=pl: e.matmul(yps[c4][:], lhsT=Cp[1][:, pr, :], rhs=nXi[xs][:, sl],
                                                                                    start=False, stop=(pl == 3)),
                     r=["Cp1", f"nXi{xs}"], w=[f"yps{c4}"])
            if pl == 3:
                for c4 in range(4):
                    sl = slice(c4 * 512, (c4 + 1) * 512)
                    P.op("vector", lambda e, ct=ct, sl=sl, c4=c4: e.scalar_tensor_tensor(
                        out=ypre[:, sl], in0=uTb[:, ct, sl], scalar=dcol[:, ct:ct + 1], in1=yps[c4][:], op0=ALU.mult, op1=ALU.add),
                        r=[f"uTb{ct}", "dcol", f"yps{c4}"], w=["Sr"])
                P.op("gpsimd", lambda e: e.tensor_tensor(out=yt2[:], in0=ypre[:], in1=ypre[:], op=ALU.mult), r=["Sr"], w=["Si"])
                P.op("gpsimd", lambda e: e.tensor_scalar(out=yt2[:], in0=yt2[:], scalar1=0.044715, scalar2=1.0, op0=ALU.mult, op1=ALU.add),
                     r=["Si"], w=["Si"])
                P.op("gpsimd", lambda e: e.tensor_tensor(out=yt2[:], in0=yt2[:], in1=ypre[:], op=ALU.mult), r=["Si", "Sr"], w=["Si"])
                P.op("scalar", lambda e: e.activation(out=yt2[:], in_=yt2[:], func=AF.Sigmoid, scale=1.5957691216057308),
                     r=["Si"], w=["Si"])
                P.op("vector", lambda e, ct=ct: e.tensor_tensor(out=yaT[:, ct, :], in0=yt2[:], in1=ypre[:], op=ALU.mult),
                     r=["Si", "Sr"], w=[f"yaT{ct}"])
        if stop == 3:
            for ct in range(4):
                P.op("vector", lambda e, ct=ct: e.tensor_copy(out=ypre[:], in_=yaT[:, ct, :]), r=[f"yaT{ct}", "Sr"], w=["Sr"])
                P.op("sync", lambda e, ct=ct: e.dma_start(out=dbg[:, ct * 2048:(ct + 1) * 2048], in_=ypre[:]), r=["Sr"], dma="dbgout")
            P.emit(final=True)
            return nc
        P.barrier()
        P.emit()
    S5.close()

    ssm_w_glu = din("ssm_w_glu", [512, 512])
    b_glu = din("b_glu", [128, 4])
    yakeys = [f"yaT{c}" for c in range(4)]
    with ExitStack() as GS:
        def sbg(name, shape, dt=F32):
            return GS.enter_context(nc.sbuf_tensor(name, list(shape), dt))
        wstg = sbg("wstg", [128, 4, 512]); wglu = sbg("wglu", [128, 4, 512], BF16); bgl = sbg("bgl", [128, 4])
        sgt = [sbg(f"sgt{i}", [128, 512]) for i in range(2)]
        P.psum.update(["pg0", "pg1"])
        pg = [GS.enter_context(nc.psum_tensor(f"pg{i}", [128, 512], F32)) for i in range(2)]
        for q in range(4):
            P.op("sync", lambda e, q=q: e.dma_start(out=wstg[:, q, :], in_=ssm_w_glu[q * 128:(q + 1) * 128, :]), w=["wstg"], dma="wstg")
        P.op("sync", lambda e: e.dma_start(out=bgl[:], in_=b_glu), w=["bgl"], dma="bgl")
        P.op("vector", lambda e: e.tensor_copy(out=wglu[:], in_=wstg[:]), r=["wstg"], w=["wglu"])
        k = 0
        for ct2 in range(4):
            for c4 in range(4):
                pz = k % 2
                k += 1
                sl = slice(c4 * 512, (c4 + 1) * 512)
                for kc in range(4):
                    P.op("tensor", lambda e, ct2=ct2, kc=kc, sl=sl, pz=pz: e.matmul(
                        pg[pz][:], lhsT=wglu[:, kc, ct2 * 128:(ct2 + 1) * 128], rhs=yaT[:, kc, sl], start=(kc == 0), stop=(kc == 3)),
                        r=["wglu"] + yakeys, w=[f"pg{pz}"])
                P.op("scalar", lambda e, ct2=ct2, pz=pz: e.activation(out=sgt[pz][:], in_=pg[pz][:], func=AF.Sigmoid,
                                                                     bias=bgl[:, ct2:ct2 + 1]),
                     r=[f"pg{pz}", "bgl"], w=[f"sgt{pz}"])
                P.op("vector", lambda e, ct2=ct2, sl=sl, pz=pz: e.tensor_tensor(out=yagT[:, ct2, sl], in0=sgt[pz][:], in1=yaT[:, ct2, sl],
                                                                               op=ALU.mult),
                     r=[f"sgt{pz}"] + yakeys, w=[f"yagT{ct2}"])
        P.barrier()
        P.emit()

    YA.close()
    ybT = MID.enter_context(nc.sbuf_tensor("ybT", [128, 4, L], BF16))
    kind = din("kind", [64, 8 * L], BF16)
    t5m_d = din("t5m", [128, 33 * 256], BF16)
    rbB_d = din("rbB", [128, 256])
    with ExitStack() as MS:
        def sbm(name, shape, dt=F32):
            return MS.enter_context(nc.sbuf_tensor(name, list(shape), dt))

        def psm(name, shape, dt=F32):
            P.psum.add(name)
            return MS.enter_context(nc.psum_tensor(name, list(shape), dt))
        qTa = sbm("qTa", [128, 8, L], BF16)
        kTa = sbm("kTa", [128, 8, L], BF16)
        vtok = sbm("vtok", [128, NT, 512], BF16)
        Bhb = sbm("Bhb", [128, 8, 256], BF16)
        rbB = sbm("rbB_s", [128, 256])
        ones_b = sbm("ones_b", [128, 128], BF16)
        kmf = sbm("kmf", [128, 8, 8]); kmT = sbm("kmT", [128, 8, 8], BF16)
        gate = [sbm(f"gate{i}", [128, 8, 8]) for i in range(2)]
        cmpt = [sbm("cmpt0", [128, 8, 8, 8])] * 2
        rank = [sbm(f"rank{i}", [128, 8, 8]) for i in range(2)]
        pexp = [sbm(f"pexp{i}", [128, 4, 128], BF16) for i in range(2)]
        rden = [sbm(f"rden{i}", [128, 128]) for i in range(2)]
        pS = [psm(f"pS{i}", [128, 4, 128]) for i in range(2)]
        pO = [psm(f"pO{i}", [128, 128]) for i in range(2)]
        pDn = [psm(f"pDn{i}", [128, 128]) for i in range(2)]
        pG = psm("pG", [128, 64])
        pM = psm("pM", [64, 128])

        P.op("sync", lambda e: e.dma_start(out=kTa[0:64, :, :].rearrange("p h t -> p (h t)"), in_=kind), w=[f"kTi{h}" for h in range(8)], dma="kind")
        P.op("sync", lambda e: e.dma_start(out=rbB[:], in_=rbB_d), w=["rbB"], dma="rbB")
        P.op("vector", lambda e: e.memset(ones_b[:], 1.0), w=["ones_b"])
        P.op("gpsimd", lambda e: e.memset(qTa[0:64, :, :], 0.0), w=[f"qTm{t}" for t in range(NT)])
        P.op("vector", lambda e: e.memset(kmT[0:64, :, :], 0.0), w=["kmT0"])
        T5S = ExitStack()
        t5m = T5S.enter_context(nc.sbuf_tensor("t5m_s", [128, 33, 256], BF16))
        Bhf = T5S.enter_context(nc.sbuf_tensor("Bhf", [128, 256], F32))
        P.op("sync", lambda e: e.dma_start(out=t5m[:].rearrange("p a b -> p (a b)"), in_=t5m_d), w=["t5m"], dma="t5m")
        for h in range(8):
            P.op("vector", lambda e: e.tensor_scalar(out=Bhf[:], in0=t5m[:, 32, :], scalar1=NEG, scalar2=None, op0=ALU.mult),
                 r=["t5m"], w=["Bhf"])
            for b in range(32):
                P.op("vector", lambda e, b=b, h=h: e.scalar_tensor_tensor(out=Bhf[:], in0=t5m[:, b, :], scalar=rbB[:, b * 8 + h:b * 8 + h + 1],
                                                                         in1=Bhf[:], op0=ALU.mult, op1=ALU.add),
                     r=["t5m", "rbB", "Bhf"], w=["Bhf"])
            P.op("vector", lambda e, h=h: e.tensor_copy(out=Bhb[:, h, :], in_=Bhf[:]), r=["Bhf"], w=[f"Bhb{h}"])

        P.barrier()
        P.emit()
        T5S.close()
        wsm = sbm("wsm", [128, 8, 256]); wbm = sbm("wbm", [128, 8, 512], BF16)
        def load_wm(c0):
            for hf in range(2):
                for q in range(8):
                    P.op("sync", lambda e, q=q, hf=hf: e.dma_start(out=wsm[:, q, :], in_=w_in[q * 128:(q + 1) * 128, c0 + hf * 256:c0 + hf * 256 + 256]),
                         w=["wsm"], dma="wsm")
                P.op("vector", lambda e, hf=hf: e.tensor_copy(out=wbm[:, :, hf * 256:(hf + 1) * 256], in_=wsm[:]), r=["wsm"], w=["wbm"])

        kk = 0

        def proj_fm(loc, dst, h, dkey, scale):
            nonlocal kk
            for c4 in range(4):
                pz = kk % 2
                kk += 1
                sl = slice(c4 * 512, (c4 + 1) * 512)
                po = pS[pz][:].rearrange("p a b -> p (a b)")
                for dk in range(8):
                    P.op("tensor", lambda e, dk=dk, sl=sl, po=po: e.matmul(po, lhsT=wbm[:, dk, loc:loc + 128], rhs=hT[:, dk, sl],
                                                                          start=(dk == 0), stop=(dk == 7)),
                         r=["wbm"] + hkeys[c4 * 4:c4 * 4 + 4], w=[f"pS{pz}"])
                P.op("scalar", lambda e, sl=sl, po=po: e.activation(out=dst[64:128, h, sl], in_=po[64:128, :], func=AF.Copy, scale=scale),
                     r=[f"pS{pz}"], w=[dkey])

        load_wm(448)
        for h in range(7):
            proj_fm(h * 64, qTa, h, f"qTq{h}", 0.125)
        load_wm(896)
        proj_fm(0, qTa, 7, "qTq7", 0.125)
        for h in range(6):
            proj_fm(64 + h * 64, kTa, h, f"kTk{h}", 1.0)
        load_wm(1344)
        proj_fm(0, kTa, 6, "kTk6", 1.0)
        proj_fm(64, kTa, 7, "kTk7", 1.0)
        load_wm(1536)
        for tt in range(NT):
            pz = kk % 2
            kk += 1
            po = pS[pz][:].rearrange("p a b -> p (a b)")
            for dk in range(8):
                P.op("tensor", lambda e, dk=dk, tt=tt, po=po: e.matmul(po, lhsT=hT[:, dk, tt * 128:(tt + 1) * 128], rhs=wbm[:, dk, :],
                                                                      start=(dk == 0), stop=(dk == 7)),
                     r=["wbm", f"hT{tt}"], w=[f"pS{pz}"])
            P.op("scalar", lambda e, tt=tt, po=po: e.copy(out=vtok[:, tt, :], in_=po), r=[f"pS{pz}"], w=[f"vtok{tt}"])

        for h in range(8):
            P.op("vector", lambda e, h=h: e.tensor_reduce(out=kmf[64:128, h, :], in_=kTa[64:128, h, :].rearrange("p (n k) -> p n k", k=256),
                                                          axis=AX.X, op=ALU.add), r=[f"kTk{h}"], w=["kmf"])
        P.op("vector", lambda e: e.tensor_scalar(out=kmT[64:128, :, :], in0=kmf[64:128, :, :], scalar1=1.0 / 256, scalar2=None, op0=ALU.mult),
             r=["kmf"], w=["kmT1"])

        for qt in range(NT):
            gz = qt % 2
            qb = qt // 2
            for h in range(8):
                P.op("tensor", lambda e, h=h, qt=qt: e.matmul(pG[:, h * 8:(h + 1) * 8], lhsT=qTa[:, h, qt * 128:(qt + 1) * 128], rhs=kmT[:, h, :],
                                                             start=True, stop=True),
                     r=[f"qTq{h}", f"qTm{qt}", "kmT0", "kmT1"], w=["pG"])
            P.op("vector", lambda e, gz=gz: e.tensor_copy(out=gate[gz][:].rearrange("p a b -> p (a b)"), in_=pG[:]), r=["pG"], w=[f"gate{gz}"])
            P.op("vector", lambda e, gz=gz, qb=qb: e.memset(gate[gz][:, :, qb:8], -1e30), w=[f"gate{gz}"])
            P.op("vector", lambda e, gz=gz: e.tensor_tensor(out=cmpt[gz][:], in0=gate[gz][:].unsqueeze(2).to_broadcast([128, 8, 8, 8]),
                                                            in1=gate[gz][:].unsqueeze(3).to_broadcast([128, 8, 8, 8]), op=ALU.is_gt),
                 r=[f"gate{gz}"], w=["cmpt0"])
            P.op("vector", lambda e, gz=gz: e.tensor_reduce(out=rank[gz][:], in_=cmpt[gz][:], axis=AX.X, op=ALU.add),
                 r=["cmpt0"], w=[f"rank{gz}"])
            P.op("vector", lambda e, gz=gz: e.tensor_scalar(out=rank[gz][:], in0=rank[gz][:], scalar1=2.5, scalar2=NEG, op0=ALU.is_gt, op1=ALU.mult),
                 r=[f"rank{gz}"], w=[f"rank{gz}"])
            P.op("vector", lambda e, gz=gz, qb=qb: e.memset(rank[gz][:, :, qb:qb + 1], 0.0), r=[f"rank{gz}"], w=[f"rank{gz}"])
            P.op("tensor", lambda e, gz=gz: e.transpose(out=pM[:], in_=rank[gz][:].rearrange("p a b -> p (a b)"), identity=ident_f[:]),
                 r=[f"rank{gz}", "ident_f"], w=["pM"])
            P.op("scalar", lambda e, qt=qt: e.copy(out=qTa[0:64, :, qt * 128:(qt + 1) * 128],
                                                   in_=pM[:].unsqueeze(1).to_broadcast([64, 8, 128])),
                 r=["pM"], w=[f"qTm{qt}"])

        gi = 0
        oi = 0
        for h in range(8):
            hp, hs = h // 2, (h % 2) * 64
            b31 = rbB[:, 31 * 8 + h:31 * 8 + h + 1]
            for qt in range(NT):
                oz = oi % 2
                oi += 1
                qsl = slice(qt * 128, (qt + 1) * 128)
                tiles = list(range(qt + 1))
                for g0 in range(0, qt + 1, 4):
                    grp = tiles[g0:g0 + 4]
                    sz = gi % 2
                    gi += 1
                    nfar = 0
                    for j, kt in enumerate(grp):
                        near = kt >= qt - 1
                        if not near:
                            nfar += 1
                        P.op("tensor", lambda e, j=j, kt=kt, h=h, qsl=qsl, sz=sz, near=near: e.matmul(
                            pS[sz][:, j, :], lhsT=kTa[:, h, kt * 128:(kt + 1) * 128], rhs=qTa[:, h, qsl], start=True, stop=(not near)),
                            r=[f"kTk{h}", f"kTi{h}", f"qTq{h}", f"qTm{qt}"], w=[f"pS{sz}"])
                        if near:
                            dlt = qt - kt
                            P.op("tensor", lambda e, j=j, h=h, dlt=dlt, sz=sz: e.matmul(
                                pS[sz][:, j, :], lhsT=ident_b[:], rhs=Bhb[:, h, dlt * 128:(dlt + 1) * 128], start=False, stop=True),
                                r=["ident_b", f"Bhb{h}"], w=[f"pS{sz}"])
                    if nfar > 0:
                        P.op("scalar", lambda e, sz=sz, nfar=nfar, b31=b31: e.activation(out=pexp[sz][:, 0:nfar, :], in_=pS[sz][:, 0:nfar, :],
                                                                                      func=AF.Exp, bias=b31),
                             r=[f"pS{sz}", "rbB"], w=[f"pexp{sz}"])
                    if nfar < len(grp):
                        P.op("scalar", lambda e, sz=sz, nfar=nfar, n=len(grp): e.activation(out=pexp[sz][:, nfar:n, :], in_=pS[sz][:, nfar:n, :],
                                                                                         func=AF.Exp),
                             r=[f"pS{sz}"], w=[f"pexp{sz}"])
                    for j, kt in enumerate(grp):
                        P.op("tensor", lambda e, j=j, kt=kt, hp=hp, sz=sz, oz=oz, qt=qt: e.matmul(
                            pO[oz][:], lhsT=vtok[:, kt, hp * 128:(hp + 1) * 128], rhs=pexp[sz][:, j, :], start=(kt == 0), stop=(kt == qt)),
                            r=[f"vtok{kt}", f"pexp{sz}"], w=[f"pO{oz}"])
                        P.op("tensor", lambda e, j=j, kt=kt, sz=sz, oz=oz, qt=qt: e.matmul(
                            pDn[oz][:], lhsT=ones_b[:], rhs=pexp[sz][:, j, :], start=(kt == 0), stop=(kt == qt)),
                            r=["ones_b", f"pexp{sz}"], w=[f"pDn{oz}"])
                P.op("vector", lambda e, oz=oz, hs=hs: e.reciprocal(out=rden[oz][hs:hs + 64, :], in_=pDn[oz][hs:hs + 64, :]),
                     r=[f"pDn{oz}"], w=[f"rden{oz}"])
                P.op("vector", lambda e, oz=oz, hs=hs, hp=hp, qsl=qsl: e.tensor_tensor(out=ybT[hs:hs + 64, hp, qsl], in0=pO[oz][hs:hs + 64, :],
                                                                                      in1=rden[oz][hs:hs + 64, :], op=ALU.mult),
                     r=[f"pO{oz}", f"rden{oz}"], w=[f"ybT{hp}"])
        if stop == 4:
            for ct in range(4):
                P.op("vector", lambda e, ct=ct: e.tensor_copy(out=wsm[:].rearrange("p a b -> p (a b)"), in_=ybT[:, ct, :]),
                     r=[f"ybT{ct}", "wsm"], w=["wsm"])
                P.op("sync", lambda e, ct=ct: e.dma_start(out=dbg[:, ct * 2048:(ct + 1) * 2048], in_=wsm[:].rearrange("p a b -> p (a b)")),
                     r=["wsm"], dma="dbgout")
            P.emit(final=True)
            return nc
        P.barrier()
        P.emit()
    w_up_ssm = din("w_up_ssm", [512, D])
    w_up_att = din("w_up_att", [512, D])
    w_out = din("w_out", [D, D])
    with ExitStack() as GM:
        def sbq(name, shape, dt=F32):
            return GM.enter_context(nc.sbuf_tensor(name, list(shape), dt))

        def psq(name, shape, dt=F32):
            P.psum.add(name)
            return GM.enter_context(nc.psum_tensor(name, list(shape), dt))
        wsg = [sbq(f"wsg{i}", [128, 8, 512]) for i in range(2)]
        wga = sbq("wga", [128, 8, 512], BF16); wgb = sbq("wgb", [128, 8, 512], BF16)
        wua = sbq("wua", [128, 4, D], BF16); wub = sbq("wub", [128, 4, D], BF16)
        sga = [sbq(f"sga{i}", [128, 512]) for i in range(2)]
        sgb = [sbq(f"sgb{i}", [128, 512]) for i in range(2)]
        m1 = [sbq(f"m1{i}", [128, 512]) for i in range(2)]
        m2 = [sbq(f"m2{i}", [128, 512]) for i in range(2)]
        pA = [psq(f"pA{i}", [128, 512]) for i in range(2)]
        pB = [psq(f"pB{i}", [128, 512]) for i in range(2)]
        pUA = [psq(f"pUA{i}", [128, 512]) for i in range(2)]
        pUB = [psq(f"pUB{i}", [128, 512]) for i in range(2)]
        for wi_, (src, dst, nm) in enumerate(((w_up_ssm, wua, "wua"), (w_up_att, wub, "wub"))):
            for half in range(2):
                for q in range(4):
                    P.op("sync", lambda e, q=q, half=half, src=src: e.dma_start(out=wsg[0][:, q, :], in_=src[q * 128:(q + 1) * 128, half * 512:(half + 1) * 512]),
                         w=["wsg0"], dma="wsg0")
                P.op("gpsimd", lambda e, half=half, dst=dst: e.tensor_copy(out=dst[:, :, half * 512:(half + 1) * 512], in_=wsg[0][:, 0:4, :]),
                     r=["wsg0"], w=[nm])
        ykeys = [f"yagT{c}" for c in range(4)]
        bkeys = [f"ybT{c}" for c in range(4)]
        k = 0
        for fg in range(2):
            for q in range(8):
                P.op("sync", lambda e, q=q, fg=fg: e.dma_start(out=wsg[0][:, q, :], in_=w_in[q * 128:(q + 1) * 128, 2048 + fg * 512:2048 + (fg + 1) * 512]),
                     w=["wsg0"], dma="wsg0")
                P.op("sync", lambda e, q=q, fg=fg: e.dma_start(out=wsg[1][:, q, :], in_=w_in[q * 128:(q + 1) * 128, 3072 + fg * 512:3072 + (fg + 1) * 512]),
                     w=["wsg1"], dma="wsg1")
            P.op("vector", lambda e: e.tensor_copy(out=wga[:], in_=wsg[0][:]), r=["wsg0"], w=["wga"])
            P.op("vector", lambda e: e.tensor_copy(out=wgb[:], in_=wsg[1][:]), r=["wsg1"], w=["wgb"])
            for fl in range(4):
                ft = fg * 4 + fl
                for c4 in range(4):
                    pz = k % 2
                    k += 1
                    sl = slice(c4 * 512, (c4 + 1) * 512)
                    fsl = slice(fl * 128, (fl + 1) * 128)
                    for dk in range(8):
                        P.op("tensor", lambda e, dk=dk, sl=sl, fsl=fsl, pz=pz: e.matmul(pA[pz][:], lhsT=wga[:, dk, fsl], rhs=hT[:, dk, sl],
                                                                                       start=(dk == 0), stop=(dk == 7)),
                             r=["wga"] + hkeys[c4 * 4:c4 * 4 + 4], w=[f"pA{pz}"])
                    for dk in range(8):
                        P.op("tensor", lambda e, dk=dk, sl=sl, fsl=fsl, pz=pz: e.matmul(pB[pz][:], lhsT=wgb[:, dk, fsl], rhs=hT[:, dk, sl],
                                                                                       start=(dk == 0), stop=(dk == 7)),
                             r=["wgb"] + hkeys[c4 * 4:c4 * 4 + 4], w=[f"pB{pz}"])
                    for kc in range(4):
                        P.op("tensor", lambda e, kc=kc, sl=sl, ft=ft, pz=pz: e.matmul(pUA[pz][:], lhsT=wua[:, kc, ft * 128:(ft + 1) * 128], rhs=yagT[:, kc, sl],
                                                                                     start=(kc == 0), stop=(kc == 3)),
                             r=["wua"] + ykeys, w=[f"pUA{pz}"])
                    for kc in range(4):
                        P.op("tensor", lambda e, kc=kc, sl=sl, ft=ft, pz=pz: e.matmul(pUB[pz][:], lhsT=wub[:, kc, ft * 128:(ft + 1) * 128], rhs=ybT[:, kc, sl],
                                                                                     start=(kc == 0), stop=(kc == 3)),
                             r=["wub"] + bkeys, w=[f"pUB{pz}"])
                    P.op("scalar", lambda e, pz=pz: e.activation(out=sga[pz][:], in_=pA[pz][:], func=AF.Sigmoid), r=[f"pA{pz}"], w=[f"sga{pz}"])
                    P.op("scalar", lambda e, pz=pz: e.activation(out=sgb[pz][:], in_=pB[pz][:], func=AF.Sigmoid), r=[f"pB{pz}"], w=[f"sgb{pz}"])
                    P.op("vector", lambda e, pz=pz: e.tensor_tensor(out=m1[pz][:], in0=sga[pz][:], in1=pUA[pz][:], op=ALU.mult),
                         r=[f"sga{pz}", f"pUA{pz}"], w=[f"m1{pz}"])
                    P.op("vector", lambda e, pz=pz: e.tensor_tensor(out=m2[pz][:], in0=sgb[pz][:], in1=pUB[pz][:], op=ALU.mult),
                         r=[f"sgb{pz}", f"pUB{pz}"], w=[f"m2{pz}"])
                    P.op("vector", lambda e, pz=pz, ft=ft, sl=sl: e.tensor_tensor(out=mergedT[:, ft, sl], in0=m1[pz][:], in1=m2[pz][:], op=ALU.add),
                         r=[f"m1{pz}", f"m2{pz}"], w=[f"mg{c4}"])
        P.barrier()
        P.emit()
    MID.close()

    TL = ExitStack()

    def sbt(name, shape, dt=F32):
        return TL.enter_context(nc.sbuf_tensor(name, list(shape), dt))

    def pst(name, shape, dt=F32):
        P.psum.add(name)
        return TL.enter_context(nc.psum_tensor(name, list(shape), dt))
    gF = sbt("gF", [128, D])
    P.op("sync", lambda e: e.dma_start(out=gF[:], in_=final_g.partition_broadcast(128)), w=["gF"], dma="gF")
    woutb = sbt("woutb", [128, 8, D], BF16)
    xr = [sbt("xr0", [128, D])] * 2
    x1 = [sbt(f"x1{i}", [128, D]) for i in range(2)]
    pX = [pst(f"pX{i}", [128, 512]) for i in range(2)]
    norm2_g = din("norm2_g", [1, D])
    peer_w_q = din("peer_w_q", [D, 2048])
    ksubT = din("ksubT", [128, 256])
    g2 = sbt("g2", [128, D])
    wqb = sbt("wqb", [128, 8, 2048], BF16)
    ktb = sbt("ktb", [128, 256], BF16)
    with ExitStack() as WS:
        wso = WS.enter_context(nc.sbuf_tensor("wso", [128, 8, 512], F32))
        ktf = WS.enter_context(nc.sbuf_tensor("ktf", [128, 256], F32))
        for half in range(2):
            for q in range(8):
                P.op("sync", lambda e, q=q, half=half: e.dma_start(out=wso[:, q, :], in_=w_out[q * 128:(q + 1) * 128, half * 512:(half + 1) * 512]),
                     w=["wso"], dma="wso")
            P.op("vector", lambda e, half=half: e.tensor_copy(out=woutb[:, :, half * 512:(half + 1) * 512], in_=wso[:]), r=["wso"], w=["woutb"])
        P.op("sync", lambda e: e.dma_start(out=g2[:], in_=norm2_g.partition_broadcast(128)), w=["g2"], dma="g2")
        P.op("sync", lambda e: e.dma_start(out=ktf[:], in_=ksubT), w=["ktf"], dma="ktf")
        P.op("vector", lambda e: e.tensor_copy(out=ktb[:], in_=ktf[:]), r=["ktf"], w=["ktb"])
        for cg in range(4):
            for q in range(8):
                P.op("sync", lambda e, q=q, cg=cg: e.dma_start(out=wso[:, q, :], in_=peer_w_q[q * 128:(q + 1) * 128, cg * 512:(cg + 1) * 512]),
                     w=["wso"], dma="wso")
            P.op("vector", lambda e, cg=cg: e.tensor_copy(out=wqb[:, :, cg * 512:(cg + 1) * 512], in_=wso[:]), r=["wso"], w=["wqb"])
        P.barrier()
        P.emit()
    st2 = sbt("st2", [128, 4])
    h2b = [sbt(f"h2b{i}", [128, D], BF16) for i in range(2)]
    h2T = sbt("h2T", [128, 8, 128], BF16)
    qpT = sbt("qpT", [128, 16, 128], BF16)
    sc = sbt("sc", [128, 16, 128]); sc2 = sc
    junk2 = sc[:].rearrange("p a b -> p (a b)")[:, 0:D]
    vv = sbt("vv", [128, 16, 16]); ix = sbt("ix", [128, 16, 16], U32); ixf = sbt("ixf", [128, 16, 16])
    ix1s = sbt("ix1s", [128, 8, 16])
    cand = sbt("cand", [128, 8, 50]); cand2 = sbt("cand2", [128, 8, 50]); cid = sbt("cid", [128, 8, 50])
    eq = sbt("eq", [128, 16, 50])
    best = sbt("best", [128, 8, 16]); nb0 = sbt("nb0", [128, 8]); Zs = sbt("Zs", [128, 8])
    gts = [sbt(f"gts{i}", [128, 8, 16]) for i in range(2)]; eid = sbt("eid", [128, 8, 16]); eidu = [sbt(f"eidu{i}", [128, 128], U32) for i in range(2)]
    aa = sbt("aa", [128, 128]); at = sbt("at", [128, 128]); wgt = sbt("wgt", [128, 128])
    NB = 16
    urow = [sbt(f"urow{i}", [128, 2 * D], BF16) for i in range(NB)]
    junkb = sbt("junkb", [128, D], BF16)
    prodb = sbt("prodb", [128, D], BF16); junkc = junkb
    yacc = xr[0]
    dgs = [sbt(f"dg{i}", [128, 128], BF16) for i in range(4)]
    pY = [pst(f"pY{i}", [128, 512]) for i in range(2)]
    pT2 = pst("pT2", [128, 8, 128], BF16)
    pQ = [pst(f"pQ{i}", [128, 4, 128]) for i in range(2)]
    pSc = pQ
    vv4 = vv[:].rearrange("p (h j) r -> p h j r", j=2)
    ixf4 = ixf[:].rearrange("p (h j) r -> p h j r", j=2)

    def rms(src, key, col, OP=None):
        OP = OP or P.op
        OP("vector", lambda e: e.memset(st2[:, col:col + 1], 0.0), w=[f"st2{col}"])
        OP("scalar", lambda e: e.activation(out=junk2, in_=src, func=AF.Square, accum_out=st2[:, col:col + 1]),
             r=[key, f"st2{col}"], w=["sc0", "sc1", f"st2{col}"])
        OP("vector", lambda e: e.tensor_scalar(out=st2[:, col:col + 1], in0=st2[:, col:col + 1], scalar1=1.0 / D, scalar2=1e-6,
                                                 op0=ALU.mult, op1=ALU.add), r=[f"st2{col}"], w=[f"st2{col}"])
        OP("scalar", lambda e: e.activation(out=st2[:, col:col + 1], in_=st2[:, col:col + 1], func=AF.Sqrt), r=[f"st2{col}"], w=[f"st2{col}"])
        OP("vector", lambda e: e.reciprocal(out=st2[:, col:col + 1], in_=st2[:, col:col + 1]), r=[f"st2{col}"], w=[f"st2{col}"])

    def make_front(tt):
        ops = []

        def Q(*a, **k):
            ops.append(lambda: P.op(*a, **k))
        s2 = tt % 2
        x1k = f"x1{s2}"
        Q("sync", lambda e, tt=tt, s2=s2: e.dma_start(out=xr[0][:], in_=x[tt * 128:(tt + 1) * 128, :]), w=["xr0"], dma="xr0")
        for half in range(2):
            for fk in range(8):
                Q("tensor", lambda e, fk=fk, tt=tt, half=half: e.matmul(pX[half][:], lhsT=mergedT[:, fk, tt * 128:(tt + 1) * 128],
                                                                          rhs=woutb[:, fk, half * 512:(half + 1) * 512], start=(fk == 0), stop=(fk == 7)),
                     r=["woutb", f"mg{tt // 4}"], w=[f"pX{half}"])
            Q("vector", lambda e, s2=s2, half=half: e.tensor_tensor(out=x1[s2][:, half * 512:(half + 1) * 512], in0=xr[0][:, half * 512:(half + 1) * 512],
                                                                       in1=pX[half][:], op=ALU.add),
                 r=["xr0", f"pX{half}"], w=[x1k])
        rms(x1[s2][:], x1k, 0, Q)
        Q("vector", lambda e, s2=s2: e.scalar_tensor_tensor(out=h2b[s2][:], in0=x1[s2][:], scalar=st2[:, 0:1], in1=g2[:], op0=ALU.mult, op1=ALU.mult),
             r=[x1k, "st20", "g2"], w=[f"h2b{s2}"])
        for dk in range(8):
            Q("tensor", lambda e, dk=dk: e.transpose(out=pT2[:, dk, :], in_=h2b[s2][:, dk * 128:(dk + 1) * 128], identity=ident_b[:]),
                 r=[f"h2b{s2}", "ident_b"], w=["pT2"])
        Q("scalar", lambda e: e.copy(out=h2T[:], in_=pT2[:]), r=["pT2"], w=["h2T"])
        for g4 in range(4):
            pz = g4 % 2
            for jj in range(4):
                hh = g4 * 4 + jj
                for dk in range(8):
                    Q("tensor", lambda e, dk=dk, hh=hh, jj=jj, pz=pz: e.matmul(pQ[pz][:, jj, :], lhsT=wqb[:, dk, hh * 128:(hh + 1) * 128], rhs=h2T[:, dk, :],
                                                                                 start=(dk == 0), stop=(dk == 7)),
                         r=["wqb", "h2T"], w=[f"pQ{pz}"])
            Q("scalar", lambda e, g4=g4, pz=pz: e.copy(out=qpT[:, g4 * 4:(g4 + 1) * 4, :], in_=pQ[pz][:]), r=[f"pQ{pz}"], w=[f"qpT{g4}"])
        for g4 in range(4):
            pz = g4 % 2
            for jj in range(4):
                hh = g4 * 4 + jj
                Q("tensor", lambda e, hh=hh, jj=jj, pz=pz: e.matmul(pSc[pz][:, jj, :], lhsT=qpT[:, hh, :], rhs=ktb[:, (hh % 2) * 128:(hh % 2) * 128 + 128],
                                                                      start=True, stop=True),
                     r=[f"qpT{g4}", "ktb"], w=[f"pQ{pz}"])
            Q("scalar", lambda e, g4=g4, pz=pz: e.copy(out=sc[:, g4 * 4:(g4 + 1) * 4, :], in_=pSc[pz][:]), r=[f"pQ{pz}"], w=[f"sc{g4}"])
        for hh in range(16):
            sk = f"sc{hh // 4}"
            Q("vector", lambda e, hh=hh: e.max(out=vv[:, hh, 0:8], in_=sc[:, hh, :]), r=[sk], w=["vv"])
            Q("vector", lambda e, hh=hh: e.max_index(out=ix[:, hh, 0:8], in_max=vv[:, hh, 0:8], in_values=sc[:, hh, :]), r=[sk, "vv"], w=["ix"])
            Q("vector", lambda e, hh=hh: e.match_replace(out=sc[:, hh, :], in_to_replace=vv[:, hh, 0:8], in_values=sc[:, hh, :], imm_value=-1e30),
                 r=[sk, "vv"], w=[sk])
            Q("vector", lambda e, hh=hh: e.max(out=vv[:, hh, 8:16], in_=sc[:, hh, :]), r=[sk], w=["vv"])
            Q("vector", lambda e, hh=hh: e.max_index(out=ix[:, hh, 8:16], in_max=vv[:, hh, 8:16], in_values=sc[:, hh, :]), r=[sk, "vv"], w=["ix"])
        Q("vector", lambda e: e.tensor_copy(out=ixf[:], in_=ix[:]), r=["ix"], w=["ixf"])
        Q("vector", lambda e: e.tensor_scalar(out=ix1s[:], in0=ixf4[:, :, 0, :], scalar1=128.0, scalar2=None, op0=ALU.mult), r=["ixf"], w=["ix1s"])
        off = 0
        for r1 in range(16):
            n = 16 // (r1 + 1)
            Q("vector", lambda e, r1=r1, n=n, off=off: e.tensor_tensor(out=cand[:, :, off:off + n], in0=vv4[:, :, 1, 0:n],
                                                                          in1=vv4[:, :, 0, r1:r1 + 1].to_broadcast([128, 8, n]), op=ALU.add),
                 r=["vv"], w=["cand"])
            Q("vector", lambda e, r1=r1, n=n, off=off: e.tensor_tensor(out=cid[:, :, off:off + n], in0=ixf4[:, :, 1, 0:n],
                                                                          in1=ix1s[:, :, r1:r1 + 1].to_broadcast([128, 8, n]), op=ALU.add),
                 r=["ixf", "ix1s"], w=["cid"])
            off += n
        assert off == 50
        for h in range(8):
            Q("vector", lambda e, h=h: e.max(out=best[:, h, 0:8], in_=cand[:, h, :]), r=["cand"], w=["best"])
            Q("vector", lambda e, h=h: e.match_replace(out=cand2[:, h, :], in_to_replace=best[:, h, 0:8], in_values=cand[:, h, :], imm_value=-1e30),
                 r=["cand", "best"], w=["cand2"])
            Q("vector", lambda e, h=h: e.max(out=best[:, h, 8:16], in_=cand2[:, h, :]), r=["cand2"], w=["best"])
        for h in range(8):
            Q("vector", lambda e, h=h: e.tensor_tensor(out=eq[:], in0=cand[:, h, :].unsqueeze(1).to_broadcast([128, 16, 50]),
                                                          in1=best[:, h, :].unsqueeze(2).to_broadcast([128, 16, 50]), op=ALU.is_equal),
                 r=["cand", "best"], w=["eq"])
            Q("vector", lambda e, h=h: e.tensor_tensor(out=eq[:], in0=eq[:], in1=cid[:, h, :].unsqueeze(1).to_broadcast([128, 16, 50]), op=ALU.mult),
                 r=["eq", "cid"], w=["eq"])
            Q("vector", lambda e, h=h: e.tensor_reduce(out=eid[:, h, :], in_=eq[:], axis=AX.X, op=ALU.max), r=["eq"], w=["eid"])
        Q("vector", lambda e: e.tensor_copy(out=eidu[s2][:], in_=eid[:].rearrange("p a b -> p (a b)")), r=["eid"], w=[f"eidu{s2}"])
        Q("vector", lambda e: e.tensor_scalar(out=nb0[:], in0=best[:, :, 0], scalar1=-1.0, scalar2=None, op0=ALU.mult), r=["best"], w=["nb0"])
        Q("vector", lambda e: e.memset(Zs[:], 0.0), w=["Zs"])
        for h in range(8):
            Q("scalar", lambda e, h=h: e.activation(out=gts[s2][:, h, :], in_=best[:, h, :], func=AF.Exp, bias=nb0[:, h:h + 1], accum_out=Zs[:, h:h + 1]),
                 r=["best", "nb0", "Zs"], w=[f"gts{s2}", "Zs"])
        Q("vector", lambda e: e.reciprocal(out=Zs[:], in_=Zs[:]), r=["Zs"], w=["Zs"])
        Q("vector", lambda e: e.tensor_tensor(out=gts[s2][:], in0=gts[s2][:], in1=Zs[:].unsqueeze(2).to_broadcast([128, 8, 16]), op=ALU.mult),
             r=[f"gts{s2}", "Zs"], w=[f"gts{s2}"])
        return ops

    cur = make_front(0)
    while cur:
        cur.pop(0)()
    for tt in range(NT):
        s2 = tt % 2
        x1k = f"x1{s2}"
        nxt = make_front(tt + 1) if tt + 1 < NT else []
        per = (len(nxt) + 15) // 16
        P.op("vector", lambda e: e.memset(aa[:], 0.0), w=["aa"])
        for g in range(16):
            gs = slice(g * 8, (g + 1) * 8)
            for jj in range(8):
                j = g * 8 + jj
                b = (g % 2) * 8 + jj
                P.op("gpsimd", lambda e, j=j, b=b, s2=s2: e.indirect_dma_start(out=urow[b][:], out_offset=None, in_=uvb,
                                                                       in_offset=bass.IndirectOffsetOnAxis(ap=eidu[s2][:, j:j + 1], axis=0)),
                     r=[f"eidu{s2}", "ubv"], w=[f"urow{b}"], dma=f"urow{b}")
                if False:
                    P.op("gpsimd", lambda e, b=b, s2=s2: e.tensor_tensor(out=prodb[:], in0=urow[b][:, 0:D], in1=h2b[s2][:], op=ALU.mult),
                         r=[f"urow{b}", f"h2b{s2}"], w=["prodb"])
                    P.op("scalar", lambda e, j=j: e.activation(out=junkc[:], in_=prodb[:], func=AF.Copy, accum_out=aa[:, j:j + 1]),
                         r=["prodb", "aa"], w=["junkb", "aa"])
                else:
                    P.op("vector", lambda e, j=j, b=b, s2=s2: e.scalar_tensor_tensor(out=junkb[:], in0=urow[b][:, 0:D], scalar=1.0, in1=h2b[s2][:], op0=ALU.mult, op1=ALU.mult,
                                                                             accum_out=aa[:, j:j + 1]),
                         r=[f"urow{b}", f"h2b{s2}", "aa"], w=["junkb", "aa"])
            P.op("vector", lambda e, gs=gs: e.tensor_tensor(out=at[:, gs], in0=aa[:, gs], in1=aa[:, gs], op=ALU.mult), r=["aa"], w=["at"])
            P.op("vector", lambda e, gs=gs: e.tensor_scalar(out=at[:, gs], in0=at[:, gs], scalar1=0.044715, scalar2=1.0, op0=ALU.mult, op1=ALU.add), r=["at"], w=["at"])
            P.op("vector", lambda e, gs=gs: e.tensor_tensor(out=at[:, gs], in0=at[:, gs], in1=aa[:, gs], op=ALU.mult), r=["at", "aa"], w=["at"])
            P.op("scalar", lambda e, gs=gs: e.activation(out=at[:, gs], in_=at[:, gs], func=AF.Sigmoid, scale=1.5957691216057308), r=["at"], w=["at"])
            for _ in range(per):
                if nxt:
                    nxt.pop(0)()
            P.op("vector", lambda e, gs=gs: e.tensor_tensor(out=at[:, gs], in0=at[:, gs], in1=aa[:, gs], op=ALU.mult), r=["at", "aa"], w=["at"])
            P.op("vector", lambda e, gs=gs, s2=s2: e.tensor_tensor(out=wgt[:, gs], in0=at[:, gs], in1=gts[s2][:].rearrange("p a b -> p (a b)")[:, gs], op=ALU.mult),
                 r=["at", f"gts{s2}"], w=["wgt"])
            for jj in range(8):
                j = g * 8 + jj
                b = (g % 2) * 8 + jj
                dz = j % 4
                P.op("scalar", lambda e, j=j, dz=dz: e.activation(out=dgs[dz][:], in_=ident_f[:], func=AF.Copy, scale=wgt[:, j:j + 1]),
                     r=["ident_f", "wgt"], w=[f"dg{dz}"])
                for half in range(2):
                    P.op("tensor", lambda e, j=j, b=b, dz=dz, half=half: e.matmul(pY[half][:], lhsT=dgs[dz][:], rhs=urow[b][:, D + half * 512:D + (half + 1) * 512],
                                                                                 start=(j == 0), stop=(j == 127)),
                         r=[f"dg{dz}", f"urow{b}"], w=[f"pY{half}"])
        while nxt:
            nxt.pop(0)()
        for half in range(2):
            P.op("vector", lambda e, half=half, s2=s2: e.tensor_tensor(out=yacc[:, half * 512:(half + 1) * 512], in0=x1[s2][:, half * 512:(half + 1) * 512],
                                                                       in1=pY[half][:], op=ALU.add),
                 r=[x1k, f"pY{half}"], w=["xr0"])
        rms(yacc[:], "xr0", 1)
        P.op("vector", lambda e: e.scalar_tensor_tensor(out=yacc[:], in0=yacc[:], scalar=st2[:, 1:2], in1=gF[:], op0=ALU.mult, op1=ALU.mult),
             r=["xr0", "st21", "gF"], w=["xr0"])
        P.op("sync", lambda e, tt=tt: e.dma_start(out=out[tt * 128:(tt + 1) * 128, :], in_=yacc[:]), r=["xr0"], dma="outst")
    P.emit(final=True)
    return nc


_NC = None


def host_inputs(inputs):
    f = lambda a: np.ascontiguousarray(np.asarray(a), dtype=np.float32)
    m = {}
    m["norm1_g"] = f(inputs["norm1_g"]).reshape(1, D)
    m["final_g"] = f(inputs["final_g"]).reshape(1, D)
    m["w_in"] = f(inputs["w_in"][0])
    are = f(inputs["ssm_a_re"][0]); aim = f(inputs["ssm_a_im"][0]); ldt = f(inputs["ssm_log_dt"][0])
    ldt_gp = np.broadcast_to(ldt[:, None], (32, 64))

    def lp(a):
        return a.reshape(16, 2, 64).transpose(1, 2, 0).reshape(128, 16)

    m["s5_lp"] = f(np.concatenate([lp(are), lp(aim), lp(ldt_gp)], axis=1))

    def row(a):
        return np.broadcast_to(a.reshape(1, 2048), (128, 2048))

    m["s5_row"] = f(np.stack([row(are), row(aim), row(ldt_gp)], axis=1))
    braw = np.zeros((2, 128, 16, 2, 64), np.float32)
    cpad = np.zeros((2, 2, 64, 16, 8, 16), np.float32)
    for ri, (bk, ck) in enumerate((("ssm_b_re", "ssm_c_re"), ("ssm_b_im", "ssm_c_im"))):
        b = f(inputs[bk][0])
        c = f(inputs[ck][0])
        for g in range(32):
            pr, g2, gl = g // 2, g % 2, g % 8
            braw[ri, gl * 16:(gl + 1) * 16, pr, g2, :] = b[g].T
            cpad[ri, g2, :, pr, gl, :] = c[g].T
    m["s5_braw"] = f(braw.reshape(2, 128, 2048).transpose(1, 0, 2))
    m["s5_cpad"] = f(cpad.reshape(2, 128, 2048).transpose(1, 0, 2))
    m["s5_d"] = f(inputs["ssm_d"][0].reshape(4, 128).T)
    m["ssm_w_glu"] = f(inputs["ssm_w_glu"][0])
    m["b_glu"] = f(inputs["ssm_b_glu"][0].reshape(4, 128).T)
    kind = np.zeros((8, 8, 8, L), np.float32)
    for h in range(8):
        for n in range(8):
            kind[h, n, h, n * 256:(n + 1) * 256] = 1.0
    m["kind"] = kind.reshape(64, 8 * L).astype(ml_dtypes.bfloat16)
    kk = np.arange(128)[:, None]
    qq = np.arange(256)[None, :]
    rel = qq - kk
    nn = np.maximum(rel, 0)
    nf = np.maximum(nn, 1).astype(np.float32)
    large = 16 + (np.log(nf / np.float32(16)) / np.float32(np.log(8.0)) * np.float32(16)).astype(np.int32)
    large = np.clip(large, 0, 31)
    bucket = np.where(nn < 16, nn, large)
    t5 = np.zeros((128, 33, 256), np.float32)
    for b in range(32):
        t5[:, b, :] = (bucket == b) & (rel >= 0)
    t5[:, 32, :] = rel < 0
    m["t5m"] = t5.reshape(128, 33 * 256).astype(ml_dtypes.bfloat16)
    m["rbB"] = f(np.broadcast_to(f(inputs["rel_bias"]).reshape(1, 256), (128, 256)))
    m["w_up_ssm"] = f(inputs["w_up_ssm"][0])
    m["w_up_att"] = f(inputs["w_up_att"][0])
    m["w_out"] = f(inputs["w_out"][0])
    m["norm2_g"] = f(inputs["norm2_g"]).reshape(1, D)
    m["peer_w_q"] = f(inputs["peer_w_q"][0])
    m["ksubT"] = f(np.concatenate([f(inputs["peer_sub_k1"][0]).T, f(inputs["peer_sub_k2"][0]).T], axis=1))
    m["peer_u"] = f(inputs["peer_u"][0])
    m["peer_v"] = f(inputs["peer_v"][0])
    m["iota_ab"] = f(np.broadcast_to(np.concatenate([np.arange(32), np.arange(64)])[None, :], (128, 96)))
    return m


def kernel(**inputs):
    global _NC
    if _NC is None:
        _NC = build()
    nc = _NC
    x = np.ascontiguousarray(inputs["x"], dtype=np.float32)
    shared = host_inputs(inputs)
    in_maps = []
    for c in range(8):
        m = dict(shared)
        m["x"] = x[c]
        in_maps.append(m)
    res = run_bass_kernel_spmd(nc, in_maps, core_ids=list(range(8)))
    return np.stack([r["out"] for r in res.results], axis=0)
```

```python
from contextlib import ExitStack
import numpy as np
import ml_dtypes
import concourse.bass as bass
import concourse.mybir as mybir
from concourse.bass_utils import run_bass_kernel_spmd

F32 = mybir.dt.float32
BF16 = mybir.dt.bfloat16
U32 = mybir.dt.uint32
ALU = mybir.AluOpType
AF = mybir.ActivationFunctionType
AX = mybir.AxisListType

D = 1024
L = 2048
NT = 16
NEG = -30000.0
PI = float(np.pi)


class Prog:
    ENGS = ("tensor", "vector", "scalar", "gpsimd", "sync")
    EPOCH = 4000
    DEPOCH = 250

    def __init__(self, nc, es):
        self.nc = nc
        self.es = es
        self.streams = {e: [] for e in self.ENGS}
        self.cnt = {e: 0 for e in self.ENGS}
        self.lastw = {}
        self.readers = {}
        self.waited = {}
        self.sems = {}
        self.dcnt = {}
        self.pending = {e: [] for e in self.ENGS}
        self.psum = set()

    def sem(self, key):
        if key not in self.sems:
            self.sems[key] = self.es.enter_context(self.nc.semaphore(f"sm{len(self.sems)}"))
        return self.sems[key]

    def _wait_for(self, sig):
        if sig[0] == "E":
            _, e2, n = sig
            ep = (n - 1) // self.EPOCH
            return ("E", e2, ep), n - ep * self.EPOCH
        _, key, n = sig
        ep = (n - 1) // self.DEPOCH
        return ("D", key, ep), 16 * (n - ep * self.DEPOCH)

    def op(self, eng, fn, r=(), w=(), dma=None):
        w = list(w) + [b for b in r if b.split(":")[0] in self.psum and b not in w]
        deps = set()
        for b in r:
            if b in self.lastw:
                deps.add(self.lastw[b])
        for b in w:
            if b in self.lastw:
                s = self.lastw[b]
                if not (s[0] == "E" and s[1] == eng and eng == "tensor"):
                    deps.add(s)
            for s in self.readers.get(b, ()):
                if not (s[0] == "E" and s[1] == eng and eng == "tensor"):
                    deps.add(s)
        waits = list(self.pending[eng])
        self.pending[eng] = []
        for s in deps:
            waits.append(self._wait_for(s))
        fw = []
        for k, v in waits:
            if self.waited.get((eng, k), 0) >= v:
                continue
            self.waited[(eng, k)] = v
            fw.append((k, v))
        if dma is None:
            self.cnt[eng] += 1
            n = self.cnt[eng]
            sig = ("E", eng, n)
            ep = (n - 1) // self.EPOCH
            inc = (("E", eng, ep), 1)
        else:
            self.dcnt[dma] = self.dcnt.get(dma, 0) + 1
            n = self.dcnt[dma]
            sig = ("D", dma, n)
            ep = (n - 1) // self.DEPOCH
            inc = (("D", dma, ep), 16)
        for k, _ in fw:
            self.sem(k)
        self.sem(inc[0])
        self.streams[eng].append((fw, fn, inc))
        for b in r:
            self.readers.setdefault(b, []).append(sig)
        for b in w:
            self.lastw[b] = sig
            self.readers[b] = []
        return sig

    def barrier(self):
        sigs = []
        for e in self.ENGS:
            if self.cnt[e] > 0:
                sigs.append(("E", e, self.cnt[e]))
        for k, n in self.dcnt.items():
            sigs.append(("D", k, n))
        for e in self.ENGS:
            for s in sigs:
                if s[0] == "E" and s[1] == e:
                    continue
                self.pending[e].append(self._wait_for(s))

    def emit(self, final=False):
        nc = self.nc
        if not final:
            return
        if final:
            self.barrier()
            for e in self.ENGS:
                self.op(e, lambda en: en.nop())
        streams = self.streams
        sems = self.sems

        def run(name, en):
            for waits, fn, inc in streams[name]:
                for k, v in waits:
                    en.wait_ge(sems[k], v)
                ins = fn(en)
                ins.then_inc(sems[inc[0]], inc[1])

        with nc.Block() as block:
            @block.tensor
            def _(en):
                run("tensor", en)

            @block.vector
            def _(en):
                run("vector", en)

            @block.scalar
            def _(en):
                run("scalar", en)

            @block.gpsimd
            def _(en):
                run("gpsimd", en)

            @block.sync
            def _(en):
                run("sync", en)
        self.streams = {e: [] for e in self.ENGS}


def build(debug=None, stop=99):
    nc = bass.Bass("TRN2", target_bir_lowering=False)
    es = ExitStack()
    P = Prog(nc, es)

    def din(name, shape, dt=F32):
        return nc.dram_tensor(name, list(shape), dt, kind="ExternalInput").ap()

    x = din("x", [L, D])
    norm1_g = din("norm1_g", [1, D])
    final_g = din("final_g", [1, D])
    out = nc.dram_tensor("out", [L, D], F32, kind="ExternalOutput").ap()
    dbg = None
    if debug is not None:
        dbg = nc.dram_tensor("dbg", list(debug), F32, kind="ExternalOutput").ap()

    def sb(name, shape, dt=F32):
        return es.enter_context(nc.sbuf_tensor(name, list(shape), dt))

    def ps(name, shape, dt=F32):
        P.psum.add(name)
        return es.enter_context(nc.psum_tensor(name, list(shape), dt))

    ident_b = sb("ident_b", [128, 128], BF16)
    ident_f = sb("ident_f", [128, 128], F32)
    ss = sb("ss", [128, NT])
    rstd = sb("rstd", [128, NT])
    mergedT = sb("mergedT", [128, 8, L], BF16)
    MID = ExitStack()
    hT = MID.enter_context(nc.sbuf_tensor("hT", [128, 8, L], BF16))
    yagT = MID.enter_context(nc.sbuf_tensor("yagT", [128, 4, L], BF16))
    P1 = ExitStack()
    g1 = P1.enter_context(nc.sbuf_tensor("g1", [128, D], F32))
    xt = [P1.enter_context(nc.sbuf_tensor(f"xt{i}", [128, D], F32)) for i in range(2)]
    junk = P1.enter_context(nc.sbuf_tensor("junk", [128, D], F32))
    hb = [P1.enter_context(nc.sbuf_tensor(f"hb{i}", [128, D], BF16)) for i in range(2)]
    P.psum.update(["pT0", "pT1"])
    pT = [P1.enter_context(nc.psum_tensor(f"pT{i}", [128, 8, 128], BF16)) for i in range(2)]

    P.op("gpsimd", lambda e: e.memset(ident_f[:], 0.0), w=["ident_f"])
    P.op("gpsimd", lambda e: e.affine_select(out=ident_f[:], in_=ident_f[:], pattern=[[-1, 128]],
                                             compare_op=ALU.not_equal, fill=1.0, base=0,
                                             channel_multiplier=1), r=["ident_f"], w=["ident_f"])
    P.op("vector", lambda e: e.tensor_copy(out=ident_b[:], in_=ident_f[:]), r=["ident_f"], w=["ident_b"])
    P.op("sync", lambda e: e.dma_start(out=g1[:], in_=norm1_g.partition_broadcast(128)), w=["g1"], dma="g1")
    P.op("vector", lambda e: e.memset(ss[:], 0.0), w=[f"ss{t}" for t in range(NT)])
    for tt in range(NT):
        s = tt % 2
        P.op("sync", lambda e, tt=tt, s=s: e.dma_start(out=xt[s][:], in_=x[tt * 128:(tt + 1) * 128, :]),
             w=[f"xt{s}"], dma=f"xt{s}")
        P.op("scalar", lambda e, tt=tt, s=s: e.activation(out=junk[:], in_=xt[s][:], func=AF.Square,
                                                          accum_out=ss[:, tt:tt + 1]),
             r=[f"xt{s}"], w=["junk", f"ss{tt}"])
        P.op("vector", lambda e, tt=tt: e.tensor_scalar(out=rstd[:, tt:tt + 1], in0=ss[:, tt:tt + 1],
                                                        scalar1=1.0 / D, scalar2=1e-6, op0=ALU.mult, op1=ALU.add),
             r=[f"ss{tt}"], w=[f"rstd{tt}"])
        P.op("scalar", lambda e, tt=tt: e.activation(out=rstd[:, tt:tt + 1], in_=rstd[:, tt:tt + 1], func=AF.Sqrt),
             r=[f"rstd{tt}"], w=[f"rstd{tt}"])
        P.op("vector", lambda e, tt=tt: e.reciprocal(out=rstd[:, tt:tt + 1], in_=rstd[:, tt:tt + 1]),
             r=[f"rstd{tt}"], w=[f"rstd{tt}"])
        P.op("vector", lambda e, tt=tt, s=s: e.scalar_tensor_tensor(out=hb[s][:], in0=xt[s][:], scalar=rstd[:, tt:tt + 1],
                                                                    in1=g1[:], op0=ALU.mult, op1=ALU.mult),
             r=[f"xt{s}", f"rstd{tt}", "g1"], w=[f"hb{s}"])
        for dk in range(8):
            P.op("tensor", lambda e, s=s, dk=dk: e.transpose(out=pT[s][:, dk, :], in_=hb[s][:, dk * 128:(dk + 1) * 128],
                                                             identity=ident_b[:]),
                 r=[f"hb{s}", "ident_b"], w=[f"pT{s}"])
        P.op("scalar", lambda e, tt=tt, s=s: e.copy(out=hT[:, :, tt * 128:(tt + 1) * 128], in_=pT[s][:]),
             r=[f"pT{s}"], w=[f"hT{tt}"])


    if stop == 1:
        for dk in range(8):
            P.op("vector", lambda e, dk=dk: e.tensor_copy(out=junk[:, 0:1024], in_=hT[:, dk, 0:1024]),
                 r=[f"hT{t}" for t in range(NT)] + ["junk"], w=["junk"])
            P.op("sync", lambda e, dk=dk: e.dma_start(out=dbg[:, dk * 1024:(dk + 1) * 1024], in_=junk[:, 0:1024]),
                 r=["junk"], w=[], dma="dbgout")
        P.emit(final=True)
        return nc
    P.barrier()
    P.emit()
    P1.close()

    w_in = din("w_in", [D, 4096])
    s5_lp = din("s5_lp", [128, 48])
    s5_row = din("s5_row", [128, 3, 2048])
    s5_braw = din("s5_braw", [128, 2, 2048])
    s5_cpad = din("s5_cpad", [128, 2, 2048])
    s5_d = din("s5_d", [128, 4])
    iota_ab = din("iota_ab", [128, 96])

    YA = ExitStack()
    yaT = YA.enter_context(nc.sbuf_tensor("yaT", [128, 4, L], BF16))
    S5 = ExitStack()

    def sbp(name, shape, dt=F32):
        return S5.enter_context(nc.sbuf_tensor(name, list(shape), dt))

    def psp(name, shape, dt=F32):
        P.psum.add(name)
        return S5.enter_context(nc.psum_tensor(name, list(shape), dt))

    Bp = [sbp(f"Bp{i}", [128, 16, 128], BF16) for i in range(2)]
    Cp = [sbp(f"Cp{i}", [128, 16, 128], BF16) for i in range(2)]
    lp = sbp("lp", [128, 48])
    rho = sbp("rho", [128, 16])
    th = sbp("th", [128, 16])
    th64 = sbp("th64", [128, 16])
    dcol = sbp("dcol", [128, 4])
    iab = sbp("iab", [128, 96])
    ang1 = sbp("ang1", [128, 16, 32])
    thb = sbp("thb", [128, 16, 64])
    MAGIC = 12582912.0
    I2PI = float(1.0 / (2 * np.pi))
    TWOPI = float(2 * np.pi)

    def reduce_angle(eng, dst, src, tmp, rk, wk, tk, shift=0.0):
        P.op(eng, lambda e: e.tensor_scalar(out=tmp, in0=src, scalar1=I2PI, scalar2=MAGIC + shift * I2PI,
                                            op0=ALU.mult, op1=ALU.add), r=rk, w=tk)
        P.op(eng, lambda e: e.tensor_scalar(out=tmp, in0=tmp, scalar1=-MAGIC, scalar2=-TWOPI,
                                            op0=ALU.add, op1=ALU.mult), r=tk, w=tk)
        P.op(eng, lambda e: e.tensor_tensor(out=dst, in0=src, in1=tmp, op=ALU.add), r=list(rk) + list(tk), w=wk)
        if shift != 0.0:
            P.op(eng, lambda e: e.tensor_scalar(out=dst, in0=dst, scalar1=shift, scalar2=None, op0=ALU.add), r=wk, w=wk)

    P.op("sync", lambda e: e.dma_start(out=lp[:], in_=s5_lp), w=["lp"], dma="lp")
    P.op("sync", lambda e: e.dma_start(out=dcol[:], in_=s5_d), w=["dcol"], dma="dcol")
    P.op("sync", lambda e: e.dma_start(out=iab[:], in_=iota_ab), w=["iab"], dma="iab")

    with ExitStack() as PR:
        def sbr(name, shape, dt=F32):
            return PR.enter_context(nc.sbuf_tensor(name, list(shape), dt))
        rowp = sbr("rowp", [128, 3, 2048])
        braw = sbr("braw", [128, 2, 2048])
        tA = sbr("tA", [128, 2048]); tB = sbr("tB", [128, 2048]); tC = sbr("tC", [128, 2048])
        tD = sbr("tD", [128, 2048]); tE = sbr("tE", [128, 2048]); tF = sbr("tF", [128, 2048])
        dtl = sbr("dtl", [128, 16]); tl = sbr("tl", [128, 16]); tl2 = sbr("tl2", [128, 16])
        P.op("sync", lambda e: e.dma_start(out=rowp[:], in_=s5_row), w=["rowp"], dma="rowp")
        P.op("sync", lambda e: e.dma_start(out=braw[:], in_=s5_braw), w=["braw"], dma="braw")
        P.op("sync", lambda e: e.dma_start(out=tE[:], in_=s5_cpad[:, 0, :]), w=["tE"], dma="crawE")
        P.op("sync", lambda e: e.dma_start(out=tF[:], in_=s5_cpad[:, 1, :]), w=["tF"], dma="crawF")
        P.op("gpsimd", lambda e: e.tensor_copy(out=Cp[0][:].rearrange("p a b -> p (a b)"), in_=tE[:]), r=["tE"], w=["Cp0"])
        P.op("gpsimd", lambda e: e.tensor_copy(out=Cp[1][:].rearrange("p a b -> p (a b)"), in_=tF[:]), r=["tF"], w=["Cp1"])
        P.op("scalar", lambda e: e.activation(out=dtl[:], in_=lp[:, 32:48], func=AF.Exp), r=["lp"], w=["dtl"])
        P.op("vector", lambda e: e.tensor_tensor(out=tl[:], in0=dtl[:], in1=lp[:, 0:16], op=ALU.mult), r=["dtl", "lp"], w=["tl"])
        P.op("scalar", lambda e: e.activation(out=rho[:], in_=tl[:], func=AF.Exp), r=["tl"], w=["rho"])
        P.op("vector", lambda e: e.tensor_tensor(out=tl[:], in0=dtl[:], in1=lp[:, 16:32], op=ALU.mult), r=["dtl", "lp", "rho"], w=["tl"])
        reduce_angle("vector", th[:], tl[:], tl2[:], ["tl"], ["th"], ["tl2"])
        P.op("vector", lambda e: e.tensor_scalar(out=tl[:], in0=th[:], scalar1=64.0, scalar2=None, op0=ALU.mult), r=["th"], w=["tl"])
        reduce_angle("vector", th64[:], tl[:], tl2[:], ["tl"], ["th64"], ["tl2"])
        for pr in range(16):
            P.op("vector", lambda e, pr=pr: e.tensor_scalar(out=ang1[:, pr, :], in0=iab[:, 0:32], scalar1=th64[:, pr:pr + 1],
                                                            scalar2=None, op0=ALU.mult), r=["iab", "th64"], w=["ang1"])
            P.op("vector", lambda e, pr=pr: e.tensor_scalar(out=thb[:, pr, :], in0=iab[:, 32:96], scalar1=th[:, pr:pr + 1],
                                                            scalar2=None, op0=ALU.mult), r=["iab", "th"], w=["thb"])
        a1f = ang1[:].rearrange("p a b -> p (a b)")
        reduce_angle("vector", a1f, a1f, tA[:, 0:512], ["ang1"], ["ang1"], ["tA"])
        are_r = rowp[:, 0, :]; aim_r = rowp[:, 1, :]
        P.op("scalar", lambda e: e.activation(out=tA[:], in_=rowp[:, 2, :], func=AF.Exp), r=["rowp", "tA"], w=["tA"])
        P.op("vector", lambda e: e.tensor_tensor(out=tB[:], in0=tA[:], in1=are_r, op=ALU.mult), r=["tA", "rowp"], w=["tB"])
        P.op("scalar", lambda e: e.activation(out=tB[:], in_=tB[:], func=AF.Exp), r=["tB"], w=["tB"])
        P.op("vector", lambda e: e.tensor_tensor(out=tC[:], in0=tA[:], in1=aim_r, op=ALU.mult), r=["tA", "rowp"], w=["tC"])
        reduce_angle("vector", tD[:], tC[:], tE[:], ["tC"], ["tD"], ["tE"])
        reduce_angle("vector", tF[:], tC[:], tE[:], ["tC"], ["tF"], ["tE"], shift=PI / 2)
        P.op("scalar", lambda e: e.activation(out=tD[:], in_=tD[:], func=AF.Sin), r=["tD"], w=["tD"])
        P.op("scalar", lambda e: e.activation(out=tF[:], in_=tF[:], func=AF.Sin), r=["tF"], w=["tF"])
        P.op("vector", lambda e: e.tensor_tensor(out=tF[:], in0=tF[:], in1=tB[:], op=ALU.mult), r=["tF", "tB"], w=["tF"])
        P.op("vector", lambda e: e.tensor_scalar(out=tF[:], in0=tF[:], scalar1=-1.0, scalar2=None, op0=ALU.add), r=["tF"], w=["tF"])
        P.op("vector", lambda e: e.tensor_tensor(out=tD[:], in0=tD[:], in1=tB[:], op=ALU.mult), r=["tD", "tB"], w=["tD"])
        P.op("vector", lambda e: e.tensor_tensor(out=tA[:], in0=are_r, in1=are_r, op=ALU.mult), r=["rowp", "tA"], w=["tA"])
        P.op("vector", lambda e: e.tensor_tensor(out=tC[:], in0=aim_r, in1=aim_r, op=ALU.mult), r=["rowp", "tC"], w=["tC"])
        P.op("vector", lambda e: e.tensor_tensor(out=tA[:], in0=tA[:], in1=tC[:], op=ALU.add), r=["tA", "tC"], w=["tA"])
        P.op("vector", lambda e: e.reciprocal(out=tA[:], in_=tA[:]), r=["tA"], w=["tA"])
        P.op("vector", lambda e: e.tensor_tensor(out=tB[:], in0=tF[:], in1=are_r, op=ALU.mult), r=["tF", "rowp", "tB"], w=["tB"])
        P.op("vector", lambda e: e.tensor_tensor(out=tE[:], in0=tD[:], in1=aim_r, op=ALU.mult), r=["tD", "rowp", "tE"], w=["tE"])
        P.op("vector", lambda e: e.tensor_tensor(out=tB[:], in0=tB[:], in1=tE[:], op=ALU.add), r=["tB", "tE"], w=["tB"])
        P.op("vector", lambda e: e.tensor_tensor(out=tB[:], in0=tB[:], in1=tA[:], op=ALU.mult), r=["tB", "tA"], w=["tB"])
        P.op("vector", lambda e: e.tensor_tensor(out=tC[:], in0=tD[:], in1=are_r, op=ALU.mult), r=["tD", "rowp", "tC"], w=["tC"])
        P.op("vector", lambda e: e.tensor_tensor(out=tE[:], in0=tF[:], in1=aim_r, op=ALU.mult), r=["tF", "rowp", "tE"], w=["tE"])
        P.op("vector", lambda e: e.tensor_tensor(out=tC[:], in0=tC[:], in1=tE[:], op=ALU.subtract), r=["tC", "tE"], w=["tC"])
        P.op("vector", lambda e: e.tensor_tensor(out=tC[:], in0=tC[:], in1=tA[:], op=ALU.mult), r=["tC", "tA"], w=["tC"])
        brr = braw[:, 0, :]; bri = braw[:, 1, :]
        P.op("vector", lambda e: e.tensor_tensor(out=tD[:], in0=brr, in1=tB[:], op=ALU.mult), r=["braw", "tB", "tD"], w=["tD"])
        P.op("vector", lambda e: e.tensor_tensor(out=tE[:], in0=bri, in1=tC[:], op=ALU.mult), r=["braw", "tC", "tE"], w=["tE"])
        P.op("vector", lambda e: e.tensor_tensor(out=Bp[0][:].rearrange("p a b -> p (a b)"), in0=tD[:], in1=tE[:], op=ALU.subtract),
             r=["tD", "tE"], w=["Bp0"])
        P.op("vector", lambda e: e.tensor_tensor(out=tD[:], in0=brr, in1=tC[:], op=ALU.mult), r=["braw", "tC", "tD"], w=["tD"])
        P.op("vector", lambda e: e.tensor_tensor(out=tE[:], in0=bri, in1=tB[:], op=ALU.mult), r=["braw", "tB", "tE"], w=["tE"])
        P.op("vector", lambda e: e.tensor_tensor(out=Bp[1][:].rearrange("p a b -> p (a b)"), in0=tD[:], in1=tE[:], op=ALU.add),
             r=["tD", "tE"], w=["Bp1"])
        if stop == 2:
            P.op("vector", lambda e: e.tensor_copy(out=tD[:], in_=Bp[0][:].rearrange("p a b -> p (a b)")), r=["Bp0", "tD"], w=["tD"])
            P.op("vector", lambda e: e.tensor_copy(out=tE[:], in_=Bp[1][:].rearrange("p a b -> p (a b)")), r=["Bp1", "tE"], w=["tE"])
            P.op("sync", lambda e: e.dma_start(out=dbg[:, 0:2048], in_=tD[:]), r=["tD"], dma="dbgout")
            P.op("sync", lambda e: e.dma_start(out=dbg[:, 2048:4096], in_=tE[:]), r=["tE"], dma="dbgout")
            P.op("sync", lambda e: e.dma_start(out=dbg[:, 4096:4096 + 16], in_=rho[:]), r=["rho"], dma="dbgout")
            P.op("sync", lambda e: e.dma_start(out=dbg[:, 4112:4112 + 16], in_=th[:]), r=["th"], dma="dbgout")
            P.op("sync", lambda e: e.dma_start(out=dbg[:, 4128:4128 + 16], in_=th64[:]), r=["th64"], dma="dbgout")
            P.op("sync", lambda e: e.dma_start(out=dbg[:, 4144:4144 + 512], in_=ang1[:].rearrange("p a b -> p (a b)")), r=["ang1"], dma="dbgout")
            P.emit(final=True)
            return nc
        P.barrier()
        P.emit()

    uTb = sbp("uTb", [128, 4, L], BF16)
    hkeys = [f"hT{t}" for t in range(NT)]
    with ExitStack() as US:
        wst = [US.enter_context(nc.sbuf_tensor(f"wst{i}", [128, 8, 512], F32)) for i in range(2)]
        wbf = [US.enter_context(nc.sbuf_tensor(f"wbf{i}", [128, 8, 512], BF16)) for i in range(2)]
        P.psum.update(["pu0", "pu1"])
        pu = [US.enter_context(nc.psum_tensor(f"pu{i}", [128, 512], F32)) for i in range(2)]

        def load_w(slot, src_ap, ncols, kc=8):
            for q in range(kc):
                P.op("sync", lambda e, q=q: e.dma_start(out=wst[slot][:, q, 0:ncols], in_=src_ap[q * 128:(q + 1) * 128, :]),
                     w=[f"wst{slot}"], dma=f"wst{slot}")
            P.op("vector", lambda e: e.tensor_copy(out=wbf[slot][:, 0:kc, 0:ncols], in_=wst[slot][:, 0:kc, 0:ncols]),
                 r=[f"wst{slot}"], w=[f"wbf{slot}"])

        load_w(0, w_in[:, 0:512], 512)
        k = 0
        for ct in range(4):
            for c4 in range(4):
                pz = k % 2
                k += 1
                for dk in range(8):
                    P.op("tensor", lambda e, ct=ct, c4=c4, dk=dk, pz=pz: e.matmul(
                        pu[pz][:], lhsT=wbf[0][:, dk, ct * 128:(ct + 1) * 128], rhs=hT[:, dk, c4 * 512:(c4 + 1) * 512],
                        start=(dk == 0), stop=(dk == 7)), r=["wbf0"] + hkeys[c4 * 4:c4 * 4 + 4], w=[f"pu{pz}"])
                P.op("scalar", lambda e, ct=ct, c4=c4, pz=pz: e.copy(out=uTb[:, ct, c4 * 512:(c4 + 1) * 512], in_=pu[pz][:]),
                     r=[f"pu{pz}"], w=[f"uTb{ct}"])
        P.barrier()
        P.emit()

    with ExitStack() as SC:
        def sbc(name, shape, dt=F32):
            return SC.enter_context(nc.sbuf_tensor(name, list(shape), dt))

        def psc(name, shape, dt=F32):
            P.psum.add(name)
            return SC.enter_context(nc.psum_tensor(name, list(shape), dt))
        scr = mergedT[:].rearrange("p a b -> p (a b)").bitcast(F32)
        sinT = [sbc("sinT", [128, 2048])[:], scr[:, 0:2048]]
        cosT = [sbc("cosT", [128, 2048])[:], scr[:, 2048:4096]]
        angE = scr[:, 4096:6144]
        tmpF = scr[:, 6144:8192]
        halfpi = sbc("halfpi", [128, 1])
        P.op("vector", lambda e: e.memset(halfpi[:], PI / 2), w=["halfpi"])
        Sr = sbc("Sr", [128, 2048]); Si = sbc("Si", [128, 2048])
        wr = sbc("wr", [128, 2048]); wi = sbc("wi", [128, 2048])
        Xr = [sbc(f"Xr{i}", [128, 2048], BF16) for i in range(2)]
        nXi = [sbc(f"nXi{i}", [128, 2048], BF16) for i in range(2)]
        ypre = Sr; yt2 = Si
        pbr = [psc(f"pbr{i}", [128, 512]) for i in range(2)]
        pbi = [psc(f"pbi{i}", [128, 512]) for i in range(2)]
        yps = [psc(f"yps{i}", [128, 512]) for i in range(4)]
        def tables_a(pr):
            tb = pr % 2
            sT, cT = sinT[tb], cosT[tb]
            sk, ck = f"sinT{tb}", f"cosT{tb}"
            P.op("vector", lambda e, pr=pr: e.tensor_tensor(
                out=angE.rearrange("p (a b) -> p a b", a=32),
                in0=ang1[:, pr, :].unsqueeze(2).to_broadcast([128, 32, 64]),
                in1=thb[:, pr, :].unsqueeze(1).to_broadcast([128, 32, 64]), op=ALU.add),
                r=["ang1", "thb"], w=["angE"])
            reduce_angle("gpsimd", sT, angE, tmpF, ["angE"], [sk], ["tmpF"])
            P.op("scalar", lambda e, sT=sT, cT=cT: e.activation(out=cT, in_=sT, func=AF.Sin, scale=0.5), r=[sk], w=[ck])
            P.op("scalar", lambda e, sT=sT: e.activation(out=sT, in_=sT, func=AF.Sin), r=[sk], w=[sk])

        def tables_b(pr):
            tb = pr % 2
            cT = cosT[tb]
            ck = f"cosT{tb}"
            P.op("vector", lambda e, cT=cT: e.tensor_tensor(out=cT, in0=cT, in1=cT, op=ALU.mult), r=[ck], w=[ck])
            P.op("vector", lambda e, cT=cT: e.tensor_scalar(out=cT, in0=cT, scalar1=-2.0, scalar2=1.0, op0=ALU.mult, op1=ALU.add), r=[ck], w=[ck])

        tables_a(0)
        tables_b(0)
        kk = 0
        for pr in range(16):
            ct, pl = pr // 4, pr % 4
            xs = pr % 2
            tb = pr % 2
            sT, cT = sinT[tb], cosT[tb]
            sk, ck = f"sinT{tb}", f"cosT{tb}"
            for c4 in range(4):
                pz = kk % 2
                kk += 1
                sl = slice(c4 * 512, (c4 + 1) * 512)
                P.op("tensor", lambda e, pr=pr, ct=ct, sl=sl, pz=pz: e.matmul(pbr[pz][:], lhsT=Bp[0][:, pr, :], rhs=uTb[:, ct, sl],
                                                                             start=True, stop=True),
                     r=["Bp0", f"uTb{ct}"], w=[f"pbr{pz}"])
                P.op("tensor", lambda e, pr=pr, ct=ct, sl=sl, pz=pz: e.matmul(pbi[pz][:], lhsT=Bp[1][:, pr, :], rhs=uTb[:, ct, sl],
                                                                             start=True, stop=True),
                     r=["Bp1", f"uTb{ct}"], w=[f"pbi{pz}"])
                P.op("scalar", lambda e, pz=pz, sl=sl: e.copy(out=Sr[:, sl], in_=pbr[pz][:]), r=[f"pbr{pz}"], w=["Sr"])
                P.op("scalar", lambda e, pz=pz, sl=sl: e.copy(out=Si[:, sl], in_=pbi[pz][:]), r=[f"pbi{pz}"], w=["Si"])
            if pr + 1 < 16:
                tables_a(pr + 1)
            P.op("vector", lambda e, cT=cT: e.tensor_tensor(out=wr[:], in0=Sr[:], in1=cT, op=ALU.mult), r=["Sr", ck], w=["wr"])
            P.op("vector", lambda e, sT=sT: e.tensor_tensor(out=wi[:], in0=Si[:], in1=sT, op=ALU.mult), r=["Si", sk], w=["wi"])
            P.op("vector", lambda e: e.tensor_tensor(out=wr[:], in0=wr[:], in1=wi[:], op=ALU.add), r=["wr", "wi"], w=["wr"])
            P.op("vector", lambda e, cT=cT: e.tensor_tensor(out=wi[:], in0=Si[:], in1=cT, op=ALU.mult), r=["Si", ck], w=["wi"])
            P.op("vector", lambda e, sT=sT: e.tensor_tensor(out=Sr[:], in0=Sr[:], in1=sT, op=ALU.mult), r=["Sr", sk], w=["Sr"])
            P.op("vector", lambda e: e.tensor_tensor(out=wi[:], in0=wi[:], in1=Sr[:], op=ALU.subtract), r=["wi", "Sr"], w=["wi"])
            rb = rho[:, pr:pr + 1].to_broadcast([128, 2048])
            P.op("vector", lambda e, rb=rb: e.tensor_tensor_scan(out=Sr[:], data0=rb, data1=wr[:], initial=0.0,
                                                                op0=ALU.mult, op1=ALU.add), r=["rho", "wr"], w=["Sr"])
            P.op("vector", lambda e, rb=rb: e.tensor_tensor_scan(out=Si[:], data0=rb, data1=wi[:], initial=0.0,
                                                                op0=ALU.mult, op1=ALU.add), r=["rho", "wi"], w=["Si"])
            P.op("vector", lambda e, cT=cT: e.tensor_tensor(out=wr[:], in0=Sr[:], in1=cT, op=ALU.mult), r=["Sr", ck], w=["wr"])
            P.op("vector", lambda e, sT=sT: e.tensor_tensor(out=wi[:], in0=Si[:], in1=sT, op=ALU.mult), r=["Si", sk], w=["wi"])
            P.op("vector", lambda e, xs=xs: e.tensor_tensor(out=Xr[xs][:], in0=wr[:], in1=wi[:], op=ALU.subtract), r=["wr", "wi"], w=[f"Xr{xs}"])
            P.op("vector", lambda e, sT=sT: e.tensor_tensor(out=wr[:], in0=Sr[:], in1=sT, op=ALU.mult), r=["Sr", sk], w=["wr"])
            P.op("vector", lambda e, cT=cT: e.tensor_tensor(out=wi[:], in0=Si[:], in1=cT, op=ALU.mult), r=["Si", ck], w=["wi"])
            P.op("vector", lambda e, xs=xs: e.scalar_tensor_tensor(out=nXi[xs][:], in0=wr[:], scalar=-1.0, in1=wi[:],
                                                                  op0=ALU.mult, op1=ALU.subtract), r=["wr", "wi"], w=[f"nXi{xs}"])
            if pr + 1 < 16:
                tables_b(pr + 1)
            for c4 in range(4):
                sl = slice(c4 * 512, (c4 + 1) * 512)
                P.op("tensor", lambda e, pr=pr, sl=sl, xs=xs, c4=c4, pl=pl: e.matmul(yps[c4][:], lhsT=Cp[0][:, pr, :], rhs=Xr[xs][:, sl],
                                                                                    start=(pl == 0), stop=False),
                     r=["Cp0", f"Xr{xs}"], w=[f"yps{c4}"])
                P.op("tensor", lambda e, pr=pr, sl=sl, xs=xs, c4=c4, pl=pl: e.matmul(yps[c4][:], lhsT=Cp[1][:, pr, :], rhs=nXi[xs][:, sl],
                                                                                    start=False, stop=(pl == 3)),
                     r=["Cp1", f"nXi{xs}"], w=[f"yps{c4}"])
            if pl == 3:
                for c4 in range(4):
                    sl = slice(c4 * 512, (c4 + 1) * 512)
                    P.op("vector", lambda e, ct=ct, sl=sl, c4=c4: e.scalar_tensor_tensor(
                        out=ypre[:, sl], in0=uTb[:, ct, sl], scalar=dcol[:, ct:ct + 1], in1=yps[c4][:], op0=ALU.mult, op1=ALU.add),
                        r=[f"uTb{ct}", "dcol", f"yps{c4}"], w=["Sr"])
                P.op("gpsimd", lambda e: e.tensor_tensor(out=yt2[:], in0=ypre[:], in1=ypre[:], op=ALU.mult), r=["Sr"], w=["Si"])
                P.op("gpsimd", lambda e: e.tensor_scalar(out=yt2[:], in0=yt2[:], scalar1=0.044715, scalar2=1.0, op0=ALU.mult, op1=ALU.add),
                     r=["Si"], w=["Si"])
                P.op("gpsimd", lambda e: e.tensor_tensor(out=yt2[:], in0=yt2[:], in1=ypre[:], op=ALU.mult), r=["Si", "Sr"], w=["Si"])
                P.op("scalar", lambda e: e.activation(out=yt2[:], in_=yt2[:], func=AF.Sigmoid, scale=1.5957691216057308),
                     r=["Si"], w=["Si"])
                P.op("vector", lambda e, ct=ct: e.tensor_tensor(out=yaT[:, ct, :], in0=yt2[:], in1=ypre[:], op=ALU.mult),
                     r=["Si", "Sr"], w=[f"yaT{ct}"])
        if stop == 3:
            for ct in range(4):
                P.op("vector", lambda e, ct=ct: e.tensor_copy(out=ypre[:], in_=yaT[:, ct, :]), r=[f"yaT{ct}", "Sr"], w=["Sr"])
                P.op("sync", lambda e, ct=ct: e.dma_start(out=dbg[:, ct * 2048:(ct + 1) * 2048], in_=ypre[:]), r=["Sr"], dma="dbgout")
            P.emit(final=True)
            return nc
        P.barrier()
        P.emit()
    S5.close()
    peer_u = din("peer_u", [16384, D])
    peer_v = din("peer_v", [16384, D])
    uvb = nc.dram_tensor("uv_scr", [16384, 2 * D], BF16, kind="Internal").ap()
    for q in range(16):
        P.op("gpsimd", lambda e, q=q: e.dma_start(out=uvb[q * 1024:(q + 1) * 1024, 0:D], in_=peer_u[q * 1024:(q + 1) * 1024, :]), w=["ubv"], dma="ubv")
        P.op("gpsimd", lambda e, q=q: e.dma_start(out=uvb[q * 1024:(q + 1) * 1024, D:2 * D], in_=peer_v[q * 1024:(q + 1) * 1024, :]), w=["ubv"], dma="ubv")


    ssm_w_glu = din("ssm_w_glu", [512, 512])
    b_glu = din("b_glu", [128, 4])
    yakeys = [f"yaT{c}" for c in range(4)]
    with ExitStack() as GS:
        def sbg(name, shape, dt=F32):
            return GS.enter_context(nc.sbuf_tensor(name, list(shape), dt))
        wstg = sbg("wstg", [128, 4, 512]); wglu = sbg("wglu", [128, 4, 512], BF16); bgl = sbg("bgl", [128, 4])
        sgt = [sbg(f"sgt{i}", [128, 512]) for i in range(2)]
        P.psum.update(["pg0", "pg1"])
        pg = [GS.enter_context(nc.psum_tensor(f"pg{i}", [128, 512], F32)) for i in range(2)]
        for q in range(4):
            P.op("sync", lambda e, q=q: e.dma_start(out=wstg[:, q, :], in_=ssm_w_glu[q * 128:(q + 1) * 128, :]), w=["wstg"], dma="wstg")
        P.op("sync", lambda e: e.dma_start(out=bgl[:], in_=b_glu), w=["bgl"], dma="bgl")
        P.op("vector", lambda e: e.tensor_copy(out=wglu[:], in_=wstg[:]), r=["wstg"], w=["wglu"])
        k = 0
        for ct2 in range(4):
            for c4 in range(4):
                pz = k % 2
                k += 1
                sl = slice(c4 * 512, (c4 + 1) * 512)
                for kc in range(4):
                    P.op("tensor", lambda e, ct2=ct2, kc=kc, sl=sl, pz=pz: e.matmul(
                        pg[pz][:], lhsT=wglu[:, kc, ct2 * 128:(ct2 + 1) * 128], rhs=yaT[:, kc, sl], start=(kc == 0), stop=(kc == 3)),
                        r=["wglu"] + yakeys, w=[f"pg{pz}"])
                P.op("scalar", lambda e, ct2=ct2, pz=pz: e.activation(out=sgt[pz][:], in_=pg[pz][:], func=AF.Sigmoid,
                                                                     bias=bgl[:, ct2:ct2 + 1]),
                     r=[f"pg{pz}", "bgl"], w=[f"sgt{pz}"])
                P.op("vector", lambda e, ct2=ct2, sl=sl, pz=pz: e.tensor_tensor(out=yagT[:, ct2, sl], in0=sgt[pz][:], in1=yaT[:, ct2, sl],
                                                                               op=ALU.mult),
                     r=[f"sgt{pz}"] + yakeys, w=[f"yagT{ct2}"])
        P.barrier()
        P.emit()

    YA.close()
    ybT = MID.enter_context(nc.sbuf_tensor("ybT", [128, 4, L], BF16))
    kind = din("kind", [64, 8 * L], BF16)
    t5m_d = din("t5m", [128, 33 * 256], BF16)
    rbB_d = din("rbB", [128, 256])
    with ExitStack() as MS:
        def sbm(name, shape, dt=F32):
            return MS.enter_context(nc.sbuf_tensor(name, list(shape), dt))

        def psm(name, shape, dt=F32):
            P.psum.add(name)
            return MS.enter_context(nc.psum_tensor(name, list(shape), dt))
        qTa = sbm("qTa", [128, 8, L], BF16)
        kTa = sbm("kTa", [128, 8, L], BF16)
        vtok = sbm("vtok", [128, NT, 512], BF16)
        Bhb = sbm("Bhb", [128, 8, 256], BF16)
        rbB = sbm("rbB_s", [128, 256])
        ones_b = sbm("ones_b", [128, 128], BF16)
        kmf = sbm("kmf", [128, 8, 8]); kmT = sbm("kmT", [128, 8, 8], BF16)
        gate = [sbm(f"gate{i}", [128, 8, 8]) for i in range(2)]
        cmpt = [sbm("cmpt0", [128, 8, 8, 8])] * 2
        rank = [sbm(f"rank{i}", [128, 8, 8]) for i in range(2)]
        pexp = [sbm(f"pexp{i}", [128, 4, 128], BF16) for i in range(2)]
        rden = [sbm(f"rden{i}", [128, 128]) for i in range(2)]
        pS = [psm(f"pS{i}", [128, 4, 128]) for i in range(2)]
        pO = [psm(f"pO{i}", [128, 128]) for i in range(2)]
        pDn = [psm(f"pDn{i}", [128, 128]) for i in range(2)]
        pG = psm("pG", [128, 64])
        pM = psm("pM", [64, 128])

        P.op("sync", lambda e: e.dma_start(out=kTa[0:64, :, :].rearrange("p h t -> p (h t)"), in_=kind), w=[f"kTi{h}" for h in range(8)], dma="kind")
        P.op("sync", lambda e: e.dma_start(out=rbB[:], in_=rbB_d), w=["rbB"], dma="rbB")
        P.op("vector", lambda e: e.memset(ones_b[:], 1.0), w=["ones_b"])
        P.op("gpsimd", lambda e: e.memset(qTa[0:64, :, :], 0.0), w=[f"qTm{t}" for t in range(NT)])
        P.op("vector", lambda e: e.memset(kmT[0:64, :, :], 0.0), w=["kmT0"])
        T5S = ExitStack()
        t5m = T5S.enter_context(nc.sbuf_tensor("t5m_s", [128, 33, 256], BF16))
        Bhf = T5S.enter_context(nc.sbuf_tensor("Bhf", [128, 256], F32))
        P.op("sync", lambda e: e.dma_start(out=t5m[:].rearrange("p a b -> p (a b)"), in_=t5m_d), w=["t5m"], dma="t5m")
        for h in range(8):
            P.op("vector", lambda e: e.tensor_scalar(out=Bhf[:], in0=t5m[:, 32, :], scalar1=NEG, scalar2=None, op0=ALU.mult),
                 r=["t5m"], w=["Bhf"])
            for b in range(32):
                P.op("vector", lambda e, b=b, h=h: e.scalar_tensor_tensor(out=Bhf[:], in0=t5m[:, b, :], scalar=rbB[:, b * 8 + h:b * 8 + h + 1],
                                                                         in1=Bhf[:], op0=ALU.mult, op1=ALU.add),
                     r=["t5m", "rbB", "Bhf"], w=["Bhf"])
            P.op("vector", lambda e, h=h: e.tensor_copy(out=Bhb[:, h, :], in_=Bhf[:]), r=["Bhf"], w=[f"Bhb{h}"])

        P.barrier()
        P.emit()
        T5S.close()
        wsm = sbm("wsm", [128, 8, 256]); wbm = sbm("wbm", [128, 8, 512], BF16)
        def load_wm(c0):
            for hf in range(2):
                for q in range(8):
                    P.op("sync", lambda e, q=q, hf=hf: e.dma_start(out=wsm[:, q, :], in_=w_in[q * 128:(q + 1) * 128, c0 + hf * 256:c0 + hf * 256 + 256]),
                         w=["wsm"], dma="wsm")
                P.op("vector", lambda e, hf=hf: e.tensor_copy(out=wbm[:, :, hf * 256:(hf + 1) * 256], in_=wsm[:]), r=["wsm"], w=["wbm"])

        kk = 0

        def proj_fm(loc, dst, h, dkey, scale):
            nonlocal kk
            for c4 in range(4):
                pz = kk % 2
                kk += 1
                sl = slice(c4 * 512, (c4 + 1) * 512)
                po = pS[pz][:].rearrange("p a b -> p (a b)")
                for dk in range(8):
                    P.op("tensor", lambda e, dk=dk, sl=sl, po=po: e.matmul(po, lhsT=wbm[:, dk, loc:loc + 128], rhs=hT[:, dk, sl],
                                                                          start=(dk == 0), stop=(dk == 7)),
                         r=["wbm"] + hkeys[c4 * 4:c4 * 4 + 4], w=[f"pS{pz}"])
                P.op("scalar", lambda e, sl=sl, po=po: e.activation(out=dst[64:128, h, sl], in_=po[64:128, :], func=AF.Copy, scale=scale),
                     r=[f"pS{pz}"], w=[dkey])

        load_wm(448)
        for h in range(7):
            proj_fm(h * 64, qTa, h, f"qTq{h}", 0.125)
        load_wm(896)
        proj_fm(0, qTa, 7, "qTq7", 0.125)
        for h in range(6):
            proj_fm(64 + h * 64, kTa, h, f"kTk{h}", 1.0)
        load_wm(1344)
        proj_fm(0, kTa, 6, "kTk6", 1.0)
        proj_fm(64, kTa, 7, "kTk7", 1.0)
        load_wm(1536)
        for tt in range(NT):
            pz = kk % 2
            kk += 1
            po = pS[pz][:].rearrange("p a b -> p (a b)")
            for dk in range(8):
                P.op("tensor", lambda e, dk=dk, tt=tt, po=po: e.matmul(po, lhsT=hT[:, dk, tt * 128:(tt + 1) * 128], rhs=wbm[:, dk, :],
                                                                      start=(dk == 0), stop=(dk == 7)),
                     r=["wbm", f"hT{tt}"], w=[f"pS{pz}"])
            P.op("scalar", lambda e, tt=tt, po=po: e.copy(out=vtok[:, tt, :], in_=po), r=[f"pS{pz}"], w=[f"vtok{tt}"])

        for h in range(8):
            P.op("vector", lambda e, h=h: e.tensor_reduce(out=kmf[64:128, h, :], in_=kTa[64:128, h, :].rearrange("p (n k) -> p n k", k=256),
                                                          axis=AX.X, op=ALU.add), r=[f"kTk{h}"], w=["kmf"])
        P.op("vector", lambda e: e.tensor_scalar(out=kmT[64:128, :, :], in0=kmf[64:128, :, :], scalar1=1.0 / 256, scalar2=None, op0=ALU.mult),
             r=["kmf"], w=["kmT1"])

        for qt in range(NT):
            gz = qt % 2
            qb = qt // 2
            for h in range(8):
                P.op("tensor", lambda e, h=h, qt=qt: e.matmul(pG[:, h * 8:(h + 1) * 8], lhsT=qTa[:, h, qt * 128:(qt + 1) * 128], rhs=kmT[:, h, :],
                                                             start=True, stop=True),
                     r=[f"qTq{h}", f"qTm{qt}", "kmT0", "kmT1"], w=["pG"])
            P.op("vector", lambda e, gz=gz: e.tensor_copy(out=gate[gz][:].rearrange("p a b -> p (a b)"), in_=pG[:]), r=["pG"], w=[f"gate{gz}"])
            P.op("vector", lambda e, gz=gz, qb=qb: e.memset(gate[gz][:, :, qb:8], -1e30), w=[f"gate{gz}"])
            P.op("vector", lambda e, gz=gz: e.tensor_tensor(out=cmpt[gz][:], in0=gate[gz][:].unsqueeze(2).to_broadcast([128, 8, 8, 8]),
                                                            in1=gate[gz][:].unsqueeze(3).to_broadcast([128, 8, 8, 8]), op=ALU.is_gt),
                 r=[f"gate{gz}"], w=["cmpt0"])
            P.op("vector", lambda e, gz=gz: e.tensor_reduce(out=rank[gz][:], in_=cmpt[gz][:], axis=AX.X, op=ALU.add),
                 r=["cmpt0"], w=[f"rank{gz}"])
            P.op("vector", lambda e, gz=gz: e.tensor_scalar(out=rank[gz][:], in0=rank[gz][:], scalar1=2.5, scalar2=NEG, op0=ALU.is_gt, op1=ALU.mult),
                 r=[f"rank{gz}"], w=[f"rank{gz}"])
            P.op("vector", lambda e, gz=gz, qb=qb: e.memset(rank[gz][:, :, qb:qb + 1], 0.0), r=[f"rank{gz}"], w=[f"rank{gz}"])
            P.op("tensor", lambda e, gz=gz: e.transpose(out=pM[:], in_=rank[gz][:].rearrange("p a b -> p (a b)"), identity=ident_f[:]),
                 r=[f"rank{gz}", "ident_f"], w=["pM"])
            P.op("scalar", lambda e, qt=qt: e.copy(out=qTa[0:64, :, qt * 128:(qt + 1) * 128],
                                                   in_=pM[:].unsqueeze(1).to_broadcast([64, 8, 128])),
                 r=["pM"], w=[f"qTm{qt}"])

        gi = 0
        oi = 0
        for h in range(8):
            hp, hs = h // 2, (h % 2) * 64
            b31 = rbB[:, 31 * 8 + h:31 * 8 + h + 1]
            for qt in range(NT):
                oz = oi % 2
                oi += 1
                qsl = slice(qt * 128, (qt + 1) * 128)
                tiles = list(range(qt + 1))
                for g0 in range(0, qt + 1, 4):
                    grp = tiles[g0:g0 + 4]
                    sz = gi % 2
                    gi += 1
                    nfar = 0
                    for j, kt in enumerate(grp):
                        near = kt >= qt - 1
                        if not near:
                            nfar += 1
                        P.op("tensor", lambda e, j=j, kt=kt, h=h, qsl=qsl, sz=sz, near=near: e.matmul(
                            pS[sz][:, j, :], lhsT=kTa[:, h, kt * 128:(kt + 1) * 128], rhs=qTa[:, h, qsl], start=True, stop=(not near)),
                            r=[f"kTk{h}", f"kTi{h}", f"qTq{h}", f"qTm{qt}"], w=[f"pS{sz}"])
                        if near:
                            dlt = qt - kt
                            P.op("tensor", lambda e, j=j, h=h, dlt=dlt, sz=sz: e.matmul(
                                pS[sz][:, j, :], lhsT=ident_b[:], rhs=Bhb[:, h, dlt * 128:(dlt + 1) * 128], start=False, stop=True),
                                r=["ident_b", f"Bhb{h}"], w=[f"pS{sz}"])
                    if nfar > 0:
                        P.op("scalar", lambda e, sz=sz, nfar=nfar, b31=b31: e.activation(out=pexp[sz][:, 0:nfar, :], in_=pS[sz][:, 0:nfar, :],
                                                                                      func=AF.Exp, bias=b31),
                             r=[f"pS{sz}", "rbB"], w=[f"pexp{sz}"])
                    if nfar < len(grp):
                        P.op("scalar", lambda e, sz=sz, nfar=nfar, n=len(grp): e.activation(out=pexp[sz][:, nfar:n, :], in_=pS[sz][:, nfar:n, :],
                                                                                         func=AF.Exp),
                             r=[f"pS{sz}"], w=[f"pexp{sz}"])
                    for j, kt in enumerate(grp):
                        P.op("tensor", lambda e, j=j, kt=kt, hp=hp, sz=sz, oz=oz, qt=qt: e.matmul(
                            pO[oz][:], lhsT=vtok[:, kt, hp * 128:(hp + 1) * 128], rhs=pexp[sz][:, j, :], start=(kt == 0), stop=(kt == qt)),
                            r=[f"vtok{kt}", f"pexp{sz}"], w=[f"pO{oz}"])
                        P.op("tensor", lambda e, j=j, kt=kt, sz=sz, oz=oz, qt=qt: e.matmul(
                            pDn[oz][:], lhsT=ones_b[:], rhs=pexp[sz][:, j, :], start=(kt == 0), stop=(kt == qt)),
                            r=["ones_b", f"pexp{sz}"], w=[f"pDn{oz}"])
                P.op("vector", lambda e, oz=oz, hs=hs: e.reciprocal(out=rden[oz][hs:hs + 64, :], in_=pDn[oz][hs:hs + 64, :]),
                     r=[f"pDn{oz}"], w=[f"rden{oz}"])
                P.op("vector", lambda e, oz=oz, hs=hs, hp=hp, qsl=qsl: e.tensor_tensor(out=ybT[hs:hs + 64, hp, qsl], in0=pO[oz][hs:hs + 64, :],
                                                                                      in1=rden[oz][hs:hs + 64, :], op=ALU.mult),
                     r=[f"pO{oz}", f"rden{oz}"], w=[f"ybT{hp}"])
        if stop == 4:
            for ct in range(4):
                P.op("vector", lambda e, ct=ct: e.tensor_copy(out=wsm[:].rearrange("p a b -> p (a b)"), in_=ybT[:, ct, :]),
                     r=[f"ybT{ct}", "wsm"], w=["wsm"])
                P.op("sync", lambda e, ct=ct: e.dma_start(out=dbg[:, ct * 2048:(ct + 1) * 2048], in_=wsm[:].rearrange("p a b -> p (a b)")),
                     r=["wsm"], dma="dbgout")
            P.emit(final=True)
            return nc
        P.barrier()
        P.emit()
    w_up_ssm = din("w_up_ssm", [512, D])
    w_up_att = din("w_up_att", [512, D])
    w_out = din("w_out", [D, D])
    with ExitStack() as GM:
        def sbq(name, shape, dt=F32):
            return GM.enter_context(nc.sbuf_tensor(name, list(shape), dt))

        def psq(name, shape, dt=F32):
            P.psum.add(name)
            return GM.enter_context(nc.psum_tensor(name, list(shape), dt))
        wsg = [sbq(f"wsg{i}", [128, 8, 512]) for i in range(2)]
        wga = sbq("wga", [128, 8, 512], BF16); wgb = sbq("wgb", [128, 8, 512], BF16)
        wua = sbq("wua", [128, 4, D], BF16); wub = sbq("wub", [128, 4, D], BF16)
        sga = [sbq(f"sga{i}", [128, 512]) for i in range(2)]
        sgb = [sbq(f"sgb{i}", [128, 512]) for i in range(2)]
        m1 = [sbq(f"m1{i}", [128, 512]) for i in range(2)]
        m2 = [sbq(f"m2{i}", [128, 512]) for i in range(2)]
        pA = [psq(f"pA{i}", [128, 512]) for i in range(2)]
        pB = [psq(f"pB{i}", [128, 512]) for i in range(2)]
        pUA = [psq(f"pUA{i}", [128, 512]) for i in range(2)]
        pUB = [psq(f"pUB{i}", [128, 512]) for i in range(2)]
        for wi_, (src, dst, nm) in enumerate(((w_up_ssm, wua, "wua"), (w_up_att, wub, "wub"))):
            for half in range(2):
                for q in range(4):
                    P.op("sync", lambda e, q=q, half=half, src=src: e.dma_start(out=wsg[0][:, q, :], in_=src[q * 128:(q + 1) * 128, half * 512:(half + 1) * 512]),
                         w=["wsg0"], dma="wsg0")
                P.op("gpsimd", lambda e, half=half, dst=dst: e.tensor_copy(out=dst[:, :, half * 512:(half + 1) * 512], in_=wsg[0][:, 0:4, :]),
                     r=["wsg0"], w=[nm])
        ykeys = [f"yagT{c}" for c in range(4)]
        bkeys = [f"ybT{c}" for c in range(4)]
        k = 0
        for fg in range(2):
            for q in range(8):
                P.op("sync", lambda e, q=q, fg=fg: e.dma_start(out=wsg[0][:, q, :], in_=w_in[q * 128:(q + 1) * 128, 2048 + fg * 512:2048 + (fg + 1) * 512]),
                     w=["wsg0"], dma="wsg0")
                P.op("sync", lambda e, q=q, fg=fg: e.dma_start(out=wsg[1][:, q, :], in_=w_in[q * 128:(q + 1) * 128, 3072 + fg * 512:3072 + (fg + 1) * 512]),
                     w=["wsg1"], dma="wsg1")
            P.op("vector", lambda e: e.tensor_copy(out=wga[:], in_=wsg[0][:]), r=["wsg0"], w=["wga"])
            P.op("vector", lambda e: e.tensor_copy(out=wgb[:], in_=wsg[1][:]), r=["wsg1"], w=["wgb"])
            for fl in range(4):
                ft = fg * 4 + fl
                for c4 in range(4):
                    pz = k % 2
                    k += 1
                    sl = slice(c4 * 512, (c4 + 1) * 512)
                    fsl = slice(fl * 128, (fl + 1) * 128)
                    for dk in range(8):
                        P.op("tensor", lambda e, dk=dk, sl=sl, fsl=fsl, pz=pz: e.matmul(pA[pz][:], lhsT=wga[:, dk, fsl], rhs=hT[:, dk, sl],
                                                                                       start=(dk == 0), stop=(dk == 7)),
                             r=["wga"] + hkeys[c4 * 4:c4 * 4 + 4], w=[f"pA{pz}"])
                    for dk in range(8):
                        P.op("tensor", lambda e, dk=dk, sl=sl, fsl=fsl, pz=pz: e.matmul(pB[pz][:], lhsT=wgb[:, dk, fsl], rhs=hT[:, dk, sl],
                                                                                       start=(dk == 0), stop=(dk == 7)),
                             r=["wgb"] + hkeys[c4 * 4:c4 * 4 + 4], w=[f"pB{pz}"])
                    for kc in range(4):
                        P.op("tensor", lambda e, kc=kc, sl=sl, ft=ft, pz=pz: e.matmul(pUA[pz][:], lhsT=wua[:, kc, ft * 128:(ft + 1) * 128], rhs=yagT[:, kc, sl],
                                                                                     start=(kc == 0), stop=(kc == 3)),
                             r=["wua"] + ykeys, w=[f"pUA{pz}"])
                    for kc in range(4):
                        P.op("tensor", lambda e, kc=kc, sl=sl, ft=ft, pz=pz: e.matmul(pUB[pz][:], lhsT=wub[:, kc, ft * 128:(ft + 1) * 128], rhs=ybT[:, kc, sl],
                                                                                     start=(kc == 0), stop=(kc == 3)),
                             r=["wub"] + bkeys, w=[f"pUB{pz}"])
                    P.op("scalar", lambda e, pz=pz: e.activation(out=sga[pz][:], in_=pA[pz][:], func=AF.Sigmoid), r=[f"pA{pz}"], w=[f"sga{pz}"])
                    P.op("scalar", lambda e, pz=pz: e.activation(out=sgb[pz][:], in_=pB[pz][:], func=AF.Sigmoid), r=[f"pB{pz}"], w=[f"sgb{pz}"])
                    P.op("vector", lambda e, pz=pz: e.tensor_tensor(out=m1[pz][:], in0=sga[pz][:], in1=pUA[pz][:], op=ALU.mult),
                         r=[f"sga{pz}", f"pUA{pz}"], w=[f"m1{pz}"])
                    P.op("vector", lambda e, pz=pz: e.tensor_tensor(out=m2[pz][:], in0=sgb[pz][:], in1=pUB[pz][:], op=ALU.mult),
                         r=[f"sgb{pz}", f"pUB{pz}"], w=[f"m2{pz}"])
                    P.op("gpsimd", lambda e, pz=pz, ft=ft, sl=sl: e.tensor_tensor(out=mergedT[:, ft, sl], in0=m1[pz][:], in1=m2[pz][:], op=ALU.add),
                         r=[f"m1{pz}", f"m2{pz}"], w=[f"mg{c4}"])
        P.barrier()
        P.emit()
    MID.close()

    TL = ExitStack()

    def sbt(name, shape, dt=F32):
        return TL.enter_context(nc.sbuf_tensor(name, list(shape), dt))

    def pst(name, shape, dt=F32):
        P.psum.add(name)
        return TL.enter_context(nc.psum_tensor(name, list(shape), dt))
    gF = sbt("gF", [128, D])
    P.op("sync", lambda e: e.dma_start(out=gF[:], in_=final_g.partition_broadcast(128)), w=["gF"], dma="gF")
    woutb = sbt("woutb", [128, 8, D], BF16)
    xr = [sbt("xr0", [128, D])] * 2
    x1 = [sbt(f"x1{i}", [128, D]) for i in range(2)]
    pX = [pst(f"pX{i}", [128, 512]) for i in range(2)]
    norm2_g = din("norm2_g", [1, D])
    peer_w_q = din("peer_w_q", [D, 2048])
    ksubT = din("ksubT", [128, 256])
    g2 = sbt("g2", [128, D])
    wqb = sbt("wqb", [128, 8, 2048], BF16)
    ktb = sbt("ktb", [128, 256], BF16)
    with ExitStack() as WS:
        wso = WS.enter_context(nc.sbuf_tensor("wso", [128, 8, 512], F32))
        ktf = WS.enter_context(nc.sbuf_tensor("ktf", [128, 256], F32))
        for half in range(2):
            for q in range(8):
                P.op("sync", lambda e, q=q, half=half: e.dma_start(out=wso[:, q, :], in_=w_out[q * 128:(q + 1) * 128, half * 512:(half + 1) * 512]),
                     w=["wso"], dma="wso")
            P.op("vector", lambda e, half=half: e.tensor_copy(out=woutb[:, :, half * 512:(half + 1) * 512], in_=wso[:]), r=["wso"], w=["woutb"])
        P.op("sync", lambda e: e.dma_start(out=g2[:], in_=norm2_g.partition_broadcast(128)), w=["g2"], dma="g2")
        P.op("sync", lambda e: e.dma_start(out=ktf[:], in_=ksubT), w=["ktf"], dma="ktf")
        P.op("vector", lambda e: e.tensor_copy(out=ktb[:], in_=ktf[:]), r=["ktf"], w=["ktb"])
        for cg in range(4):
            for q in range(8):
                P.op("sync", lambda e, q=q, cg=cg: e.dma_start(out=wso[:, q, :], in_=peer_w_q[q * 128:(q + 1) * 128, cg * 512:(cg + 1) * 512]),
                     w=["wso"], dma="wso")
            P.op("vector", lambda e, cg=cg: e.tensor_copy(out=wqb[:, :, cg * 512:(cg + 1) * 512], in_=wso[:]), r=["wso"], w=["wqb"])
        P.barrier()
        P.emit()
    st2 = sbt("st2", [128, 4])
    h2b = [sbt(f"h2b{i}", [128, D], BF16) for i in range(2)]
    h2T = sbt("h2T", [128, 8, 128], BF16)
    qpT = sbt("qpT", [128, 16, 128], BF16)
    sc = sbt("sc", [128, 16, 128]); sc2 = sc
    junk2 = sc[:].rearrange("p a b -> p (a b)")[:, 0:D]
    vv = sbt("vv", [128, 16, 16]); ix = sbt("ix", [128, 16, 16], U32); ixf = sbt("ixf", [128, 16, 16])
    ix1s = sbt("ix1s", [128, 8, 16])
    cand = sbt("cand", [128, 8, 50]); cand2 = sbt("cand2", [128, 8, 50]); cid = sbt("cid", [128, 8, 50])
    eq = sbt("eq", [128, 16, 50])
    best = sbt("best", [128, 8, 16]); nb0 = sbt("nb0", [128, 8]); Zs = sbt("Zs", [128, 8])
    gts = [sbt(f"gts{i}", [128, 8, 16]) for i in range(2)]; eid = sbt("eid", [128, 8, 16]); eidu = [sbt(f"eidu{i}", [128, 128], U32) for i in range(2)]
    aa = sbt("aa", [128, 128]); at = sbt("at", [128, 128]); wgt = sbt("wgt", [128, 128])
    NB = 16
    urow = [sbt(f"urow{i}", [128, 2 * D], BF16) for i in range(NB)]
    junkb = sbt("junkb", [128, D], BF16)
    prodb = sbt("prodb", [128, D], BF16); junkc = junkb
    yacc = xr[0]
    dgs = [sbt(f"dg{i}", [128, 128], BF16) for i in range(4)]
    pY = [pst(f"pY{i}", [128, 512]) for i in range(2)]
    pT2 = pst("pT2", [128, 8, 128], BF16)
    pQ = [pst(f"pQ{i}", [128, 4, 128]) for i in range(2)]
    pSc = pQ
    vv4 = vv[:].rearrange("p (h j) r -> p h j r", j=2)
    ixf4 = ixf[:].rearrange("p (h j) r -> p h j r", j=2)

    def rms(src, key, col, OP=None):
        OP = OP or P.op
        OP("vector", lambda e: e.memset(st2[:, col:col + 1], 0.0), w=[f"st2{col}"])
        OP("scalar", lambda e: e.activation(out=junk2, in_=src, func=AF.Square, accum_out=st2[:, col:col + 1]),
             r=[key, f"st2{col}"], w=["sc0", "sc1", f"st2{col}"])
        OP("vector", lambda e: e.tensor_scalar(out=st2[:, col:col + 1], in0=st2[:, col:col + 1], scalar1=1.0 / D, scalar2=1e-6,
                                                 op0=ALU.mult, op1=ALU.add), r=[f"st2{col}"], w=[f"st2{col}"])
        OP("scalar", lambda e: e.activation(out=st2[:, col:col + 1], in_=st2[:, col:col + 1], func=AF.Sqrt), r=[f"st2{col}"], w=[f"st2{col}"])
        OP("vector", lambda e: e.reciprocal(out=st2[:, col:col + 1], in_=st2[:, col:col + 1]), r=[f"st2{col}"], w=[f"st2{col}"])

    def make_front(tt):
        ops = []

        def Q(*a, **k):
            ops.append(lambda: P.op(*a, **k))
        s2 = tt % 2
        x1k = f"x1{s2}"
        Q("sync", lambda e, tt=tt, s2=s2: e.dma_start(out=xr[0][:], in_=x[tt * 128:(tt + 1) * 128, :]), w=["xr0"], dma="xr0")
        for half in range(2):
            for fk in range(8):
                Q("tensor", lambda e, fk=fk, tt=tt, half=half: e.matmul(pX[half][:], lhsT=mergedT[:, fk, tt * 128:(tt + 1) * 128],
                                                                          rhs=woutb[:, fk, half * 512:(half + 1) * 512], start=(fk == 0), stop=(fk == 7)),
                     r=["woutb", f"mg{tt // 4}"], w=[f"pX{half}"])
            Q("vector", lambda e, s2=s2, half=half: e.tensor_tensor(out=x1[s2][:, half * 512:(half + 1) * 512], in0=xr[0][:, half * 512:(half + 1) * 512],
                                                                       in1=pX[half][:], op=ALU.add),
                 r=["xr0", f"pX{half}"], w=[x1k])
        rms(x1[s2][:], x1k, 0, Q)
        Q("vector", lambda e, s2=s2: e.scalar_tensor_tensor(out=h2b[s2][:], in0=x1[s2][:], scalar=st2[:, 0:1], in1=g2[:], op0=ALU.mult, op1=ALU.mult),
             r=[x1k, "st20", "g2"], w=[f"h2b{s2}"])
        for dk in range(8):
            Q("tensor", lambda e, dk=dk: e.transpose(out=pT2[:, dk, :], in_=h2b[s2][:, dk * 128:(dk + 1) * 128], identity=ident_b[:]),
                 r=[f"h2b{s2}", "ident_b"], w=["pT2"])
        Q("scalar", lambda e: e.copy(out=h2T[:], in_=pT2[:]), r=["pT2"], w=["h2T"])
        for g4 in range(4):
            pz = g4 % 2
            for jj in range(4):
                hh = g4 * 4 + jj
                for dk in range(8):
                    Q("tensor", lambda e, dk=dk, hh=hh, jj=jj, pz=pz: e.matmul(pQ[pz][:, jj, :], lhsT=wqb[:, dk, hh * 128:(hh + 1) * 128], rhs=h2T[:, dk, :],
                                                                                 start=(dk == 0), stop=(dk == 7)),
                         r=["wqb", "h2T"], w=[f"pQ{pz}"])
            Q("scalar", lambda e, g4=g4, pz=pz: e.copy(out=qpT[:, g4 * 4:(g4 + 1) * 4, :], in_=pQ[pz][:]), r=[f"pQ{pz}"], w=[f"qpT{g4}"])
        for g4 in range(4):
            pz = g4 % 2
            for jj in range(4):
                hh = g4 * 4 + jj
                Q("tensor", lambda e, hh=hh, jj=jj, pz=pz: e.matmul(pSc[pz][:, jj, :], lhsT=qpT[:, hh, :], rhs=ktb[:, (hh % 2) * 128:(hh % 2) * 128 + 128],
                                                                      start=True, stop=True),
                     r=[f"qpT{g4}", "ktb"], w=[f"pQ{pz}"])
            Q("scalar", lambda e, g4=g4, pz=pz: e.copy(out=sc[:, g4 * 4:(g4 + 1) * 4, :], in_=pSc[pz][:]), r=[f"pQ{pz}"], w=[f"sc{g4}"])
        for hh in range(16):
            sk = f"sc{hh // 4}"
            Q("vector", lambda e, hh=hh: e.max(out=vv[:, hh, 0:8], in_=sc[:, hh, :]), r=[sk], w=["vv"])
            Q("vector", lambda e, hh=hh: e.max_index(out=ix[:, hh, 0:8], in_max=vv[:, hh, 0:8], in_values=sc[:, hh, :]), r=[sk, "vv"], w=["ix"])
            Q("vector", lambda e, hh=hh: e.match_replace(out=sc[:, hh, :], in_to_replace=vv[:, hh, 0:8], in_values=sc[:, hh, :], imm_value=-1e30),
                 r=[sk, "vv"], w=[sk])
            Q("vector", lambda e, hh=hh: e.max(out=vv[:, hh, 8:16], in_=sc[:, hh, :]), r=[sk], w=["vv"])
            Q("vector", lambda e, hh=hh: e.max_index(out=ix[:, hh, 8:16], in_max=vv[:, hh, 8:16], in_values=sc[:, hh, :]), r=[sk, "vv"], w=["ix"])
        Q("vector", lambda e: e.tensor_copy(out=ixf[:], in_=ix[:]), r=["ix"], w=["ixf"])
        Q("vector", lambda e: e.tensor_scalar(out=ix1s[:], in0=ixf4[:, :, 0, :], scalar1=128.0, scalar2=None, op0=ALU.mult), r=["ixf"], w=["ix1s"])
        off = 0
        for r1 in range(16):
            n = 16 // (r1 + 1)
            Q("vector", lambda e, r1=r1, n=n, off=off: e.tensor_tensor(out=cand[:, :, off:off + n], in0=vv4[:, :, 1, 0:n],
                                                                          in1=vv4[:, :, 0, r1:r1 + 1].to_broadcast([128, 8, n]), op=ALU.add),
                 r=["vv"], w=["cand"])
            Q("vector", lambda e, r1=r1, n=n, off=off: e.tensor_tensor(out=cid[:, :, off:off + n], in0=ixf4[:, :, 1, 0:n],
                                                                          in1=ix1s[:, :, r1:r1 + 1].to_broadcast([128, 8, n]), op=ALU.add),
                 r=["ixf", "ix1s"], w=["cid"])
            off += n
        assert off == 50
        for h in range(8):
            Q("vector", lambda e, h=h: e.max(out=best[:, h, 0:8], in_=cand[:, h, :]), r=["cand"], w=["best"])
            Q("vector", lambda e, h=h: e.match_replace(out=cand2[:, h, :], in_to_replace=best[:, h, 0:8], in_values=cand[:, h, :], imm_value=-1e30),
                 r=["cand", "best"], w=["cand2"])
            Q("vector", lambda e, h=h: e.max(out=best[:, h, 8:16], in_=cand2[:, h, :]), r=["cand2"], w=["best"])
        for h in range(8):
            Q("vector", lambda e, h=h: e.tensor_tensor(out=eq[:], in0=cand[:, h, :].unsqueeze(1).to_broadcast([128, 16, 50]),
                                                          in1=best[:, h, :].unsqueeze(2).to_broadcast([128, 16, 50]), op=ALU.is_equal),
                 r=["cand", "best"], w=["eq"])
            Q("vector", lambda e, h=h: e.tensor_tensor(out=eq[:], in0=eq[:], in1=cid[:, h, :].unsqueeze(1).to_broadcast([128, 16, 50]), op=ALU.mult),
                 r=["eq", "cid"], w=["eq"])
            Q("vector", lambda e, h=h: e.tensor_reduce(out=eid[:, h, :], in_=eq[:], axis=AX.X, op=ALU.max), r=["eq"], w=["eid"])
        Q("vector", lambda e: e.tensor_copy(out=eidu[s2][:], in_=eid[:].rearrange("p a b -> p (a b)")), r=["eid"], w=[f"eidu{s2}"])
        Q("vector", lambda e: e.tensor_scalar(out=nb0[:], in0=best[:, :, 0], scalar1=-1.0, scalar2=None, op0=ALU.mult), r=["best"], w=["nb0"])
        Q("vector", lambda e: e.memset(Zs[:], 0.0), w=["Zs"])
        for h in range(8):
            Q("scalar", lambda e, h=h: e.activation(out=gts[s2][:, h, :], in_=best[:, h, :], func=AF.Exp, bias=nb0[:, h:h + 1], accum_out=Zs[:, h:h + 1]),
                 r=["best", "nb0", "Zs"], w=[f"gts{s2}", "Zs"])
        Q("vector", lambda e: e.reciprocal(out=Zs[:], in_=Zs[:]), r=["Zs"], w=["Zs"])
        Q("vector", lambda e: e.tensor_tensor(out=gts[s2][:], in0=gts[s2][:], in1=Zs[:].unsqueeze(2).to_broadcast([128, 8, 16]), op=ALU.mult),
             r=[f"gts{s2}", "Zs"], w=[f"gts{s2}"])
        return ops

    cur = make_front(0)
    while cur:
        cur.pop(0)()
    for tt in range(NT):
        s2 = tt % 2
        x1k = f"x1{s2}"
        nxt = make_front(tt + 1) if tt + 1 < NT else []
        per = (len(nxt) + 15) // 16
        P.op("vector", lambda e: e.memset(aa[:], 0.0), w=["aa"])
        for g in range(16):
            gs = slice(g * 8, (g + 1) * 8)
            for jj in range(8):
                j = g * 8 + jj
                b = (g % 2) * 8 + jj
                P.op("gpsimd", lambda e, j=j, b=b, s2=s2: e.indirect_dma_start(out=urow[b][:], out_offset=None, in_=uvb,
                                                                       in_offset=bass.IndirectOffsetOnAxis(ap=eidu[s2][:, j:j + 1], axis=0)),
                     r=[f"eidu{s2}", "ubv"], w=[f"urow{b}"], dma=f"urow{b}")
                if False:
                    P.op("gpsimd", lambda e, b=b, s2=s2: e.tensor_tensor(out=prodb[:], in0=urow[b][:, 0:D], in1=h2b[s2][:], op=ALU.mult),
                         r=[f"urow{b}", f"h2b{s2}"], w=["prodb"])
                    P.op("scalar", lambda e, j=j: e.activation(out=junkc[:], in_=prodb[:], func=AF.Copy, accum_out=aa[:, j:j + 1]),
                         r=["prodb", "aa"], w=["junkb", "aa"])
                else:
                    P.op("vector", lambda e, j=j, b=b, s2=s2: e.scalar_tensor_tensor(out=junkb[:], in0=urow[b][:, 0:D], scalar=1.0, in1=h2b[s2][:], op0=ALU.mult, op1=ALU.mult,
                                                                             accum_out=aa[:, j:j + 1]),
                         r=[f"urow{b}", f"h2b{s2}", "aa"], w=["junkb", "aa"])
            P.op("vector", lambda e, gs=gs: e.tensor_tensor(out=at[:, gs], in0=aa[:, gs], in1=aa[:, gs], op=ALU.mult), r=["aa"], w=["at"])
            P.op("vector", lambda e, gs=gs: e.tensor_scalar(out=at[:, gs], in0=at[:, gs], scalar1=0.044715, scalar2=1.0, op0=ALU.mult, op1=ALU.add), r=["at"], w=["at"])
            P.op("vector", lambda e, gs=gs: e.tensor_tensor(out=at[:, gs], in0=at[:, gs], in1=aa[:, gs], op=ALU.mult), r=["at", "aa"], w=["at"])
            P.op("scalar", lambda e, gs=gs: e.activation(out=at[:, gs], in_=at[:, gs], func=AF.Sigmoid, scale=1.5957691216057308), r=["at"], w=["at"])
            for _ in range(per):
                if nxt:
                    nxt.pop(0)()
            P.op("vector", lambda e, gs=gs: e.tensor_tensor(out=at[:, gs], in0=at[:, gs], in1=aa[:, gs], op=ALU.mult), r=["at", "aa"], w=["at"])
            P.op("vector", lambda e, gs=gs, s2=s2: e.tensor_tensor(out=wgt[:, gs], in0=at[:, gs], in1=gts[s2][:].rearrange("p a b -> p (a b)")[:, gs], op=ALU.mult),
                 r=["at", f"gts{s2}"], w=["wgt"])
            for jj in range(8):
                j = g * 8 + jj
                b = (g % 2) * 8 + jj
                dz = j % 4
                P.op("scalar", lambda e, j=j, dz=dz: e.activation(out=dgs[dz][:], in_=ident_f[:], func=AF.Copy, scale=wgt[:, j:j + 1]),
                     r=["ident_f", "wgt"], w=[f"dg{dz}"])
                for half in range(2):
                    P.op("tensor", lambda e, j=j, b=b, dz=dz, half=half: e.matmul(pY[half][:], lhsT=dgs[dz][:], rhs=urow[b][:, D + half * 512:D + (half + 1) * 512],
                                                                                 start=(j == 0), stop=(j == 127)),
                         r=[f"dg{dz}", f"urow{b}"], w=[f"pY{half}"])
        while nxt:
            nxt.pop(0)()
        for half in range(2):
            P.op("vector", lambda e, half=half, s2=s2: e.tensor_tensor(out=yacc[:, half * 512:(half + 1) * 512], in0=x1[s2][:, half * 512:(half + 1) * 512],
                                                                       in1=pY[half][:], op=ALU.add),
                 r=[x1k, f"pY{half}"], w=["xr0"])
        rms(yacc[:], "xr0", 1)
        P.op("vector", lambda e: e.scalar_tensor_tensor(out=yacc[:], in0=yacc[:], scalar=st2[:, 1:2], in1=gF[:], op0=ALU.mult, op1=ALU.mult),
             r=["xr0", "st21", "gF"], w=["xr0"])
        P.op("sync", lambda e, tt=tt: e.dma_start(out=out[tt * 128:(tt + 1) * 128, :], in_=yacc[:]), r=["xr0"], dma="outst")
    P.emit(final=True)
    return nc


_NC = None


def host_inputs(inputs):
    f = lambda a: np.ascontiguousarray(np.asarray(a), dtype=np.float32)
    m = {}
    m["norm1_g"] = f(inputs["norm1_g"]).reshape(1, D)
    m["final_g"] = f(inputs["final_g"]).reshape(1, D)
    m["w_in"] = f(inputs["w_in"][0])
    are = f(inputs["ssm_a_re"][0]); aim = f(inputs["ssm_a_im"][0]); ldt = f(inputs["ssm_log_dt"][0])
    ldt_gp = np.broadcast_to(ldt[:, None], (32, 64))

    def lp(a):
        return a.reshape(16, 2, 64).transpose(1, 2, 0).reshape(128, 16)

    m["s5_lp"] = f(np.concatenate([lp(are), lp(aim), lp(ldt_gp)], axis=1))

    def row(a):
        return np.broadcast_to(a.reshape(1, 2048), (128, 2048))

    m["s5_row"] = f(np.stack([row(are), row(aim), row(ldt_gp)], axis=1))
    braw = np.zeros((2, 128, 16, 2, 64), np.float32)
    cpad = np.zeros((2, 2, 64, 16, 8, 16), np.float32)
    for ri, (bk, ck) in enumerate((("ssm_b_re", "ssm_c_re"), ("ssm_b_im", "ssm_c_im"))):
        b = f(inputs[bk][0])
        c = f(inputs[ck][0])
        for g in range(32):
            pr, g2, gl = g // 2, g % 2, g % 8
            braw[ri, gl * 16:(gl + 1) * 16, pr, g2, :] = b[g].T
            cpad[ri, g2, :, pr, gl, :] = c[g].T
    m["s5_braw"] = f(braw.reshape(2, 128, 2048).transpose(1, 0, 2))
    m["s5_cpad"] = f(cpad.reshape(2, 128, 2048).transpose(1, 0, 2))
    m["s5_d"] = f(inputs["ssm_d"][0].reshape(4, 128).T)
    m["ssm_w_glu"] = f(inputs["ssm_w_glu"][0])
    m["b_glu"] = f(inputs["ssm_b_glu"][0].reshape(4, 128).T)
    kind = np.zeros((8, 8, 8, L), np.float32)
    for h in range(8):
        for n in range(8):
            kind[h, n, h, n * 256:(n + 1) * 256] = 1.0
    m["kind"] = kind.reshape(64, 8 * L).astype(ml_dtypes.bfloat16)
    kk = np.arange(128)[:, None]
    qq = np.arange(256)[None, :]
    rel = qq - kk
    nn = np.maximum(rel, 0)
    nf = np.maximum(nn, 1).astype(np.float32)
    large = 16 + (np.log(nf / np.float32(16)) / np.float32(np.log(8.0)) * np.float32(16)).astype(np.int32)
    large = np.clip(large, 0, 31)
    bucket = np.where(nn < 16, nn, large)
    t5 = np.zeros((128, 33, 256), np.float32)
    for b in range(32):
        t5[:, b, :] = (bucket == b) & (rel >= 0)
    t5[:, 32, :] = rel < 0
    m["t5m"] = t5.reshape(128, 33 * 256).astype(ml_dtypes.bfloat16)
    m["rbB"] = f(np.broadcast_to(f(inputs["rel_bias"]).reshape(1, 256), (128, 256)))
    m["w_up_ssm"] = f(inputs["w_up_ssm"][0])
    m["w_up_att"] = f(inputs["w_up_att"][0])
    m["w_out"] = f(inputs["w_out"][0])
    m["norm2_g"] = f(inputs["norm2_g"]).reshape(1, D)
    m["peer_w_q"] = f(inputs["peer_w_q"][0])
    m["ksubT"] = f(np.concatenate([f(inputs["peer_sub_k1"][0]).T, f(inputs["peer_sub_k2"][0]).T], axis=1))
    m["peer_u"] = f(inputs["peer_u"][0])
    m["peer_v"] = f(inputs["peer_v"][0])
    m["iota_ab"] = f(np.broadcast_to(np.concatenate([np.arange(32), np.arange(64)])[None, :], (128, 96)))
    return m


def kernel(**inputs):
    global _NC
    if _NC is None:
        _NC = build()
    nc = _NC
    x = np.ascontiguousarray(inputs["x"], dtype=np.float32)
    shared = host_inputs(inputs)
    in_maps = []
    for c in range(8):
        m = dict(shared)
        m["x"] = x[c]
        in_maps.append(m)
    res = run_bass_kernel_spmd(nc, in_maps, core_ids=list(range(8)))
    return np.stack([r["out"] for r in res.results], axis=0)
```
